# Optimizing a Trainium2 kernel written in Bass

```python
import jax, jax.numpy as jnp
from jax import lax
import numpy as np

D_MODEL = 2048
BATCH = 16
SEQ = 2048
DEPTH = 2

GRID_W = 64
CTX_LEN = 256

D_MIX = D_MODEL
HEAD_DIM = 128
N_HEADS = 8
N_KV_HEADS = 2
GQA_GROUP = N_HEADS // N_KV_HEADS
D_ATTN = N_HEADS * HEAD_DIM
D_KV = N_KV_HEADS * HEAD_DIM
D_SCONV = D_MIX // 4
D_CONF = D_MIX - D_ATTN - D_SCONV
SCONV_WIDTH = 3
CONF_WIDTH = 31
Q_BLOCK = 128
ROPE_THETA = 10000.0

D_IN_PROJ = D_ATTN + 2 * D_KV + 3 * D_SCONV + 2 * D_CONF
IN_SPLITS = (
    D_ATTN,
    D_ATTN + D_KV,
    D_ATTN + 2 * D_KV,
    D_ATTN + 2 * D_KV + D_SCONV,
    D_ATTN + 2 * D_KV + 2 * D_SCONV,
    D_ATTN + 2 * D_KV + 3 * D_SCONV,
    D_ATTN + 2 * D_KV + 3 * D_SCONV + D_CONF,
)

N_GROUPS = 4
EXPERTS_PER_GROUP = 8
N_EXPERTS = N_GROUPS * EXPERTS_PER_GROUP
TOP_K = 2
D_EXPERT = D_MODEL // 4
EXPERT_BLOCK = 256

N_MOD = 6
EPS = 1e-6

kernel_name = "hybrid_parallel_groups_dit_hmoe"


def rms_norm(x, g):
    xf = x.astype(jnp.float32)
    y = xf * lax.rsqrt(jnp.mean(jnp.square(xf), axis=-1, keepdims=True) + EPS)
    return y.astype(x.dtype) * g


def layer_norm(x, g, b):
    xf = x.astype(jnp.float32)
    mu = jnp.mean(xf, axis=-1, keepdims=True)
    var = jnp.mean(jnp.square(xf - mu), axis=-1, keepdims=True)
    return ((xf - mu) * lax.rsqrt(var + EPS)).astype(x.dtype) * g + b


def modulate(h, shift, scale):
    return h * (1 + scale) + shift


def heads(t, n):
    return t.reshape(t.shape[:-1] + (n, HEAD_DIM))


def axial_rope_tables(rows):
    row = jnp.broadcast_to(jnp.arange(rows, dtype=jnp.int32)[:, None], (rows, GRID_W)).reshape(-1)
    col = jnp.broadcast_to(jnp.arange(GRID_W, dtype=jnp.int32)[None, :], (rows, GRID_W)).reshape(-1)
    axis_dim = HEAD_DIM // 2
    inv_freq = ROPE_THETA ** (-jnp.arange(0, axis_dim, 2, dtype=jnp.float32) / axis_dim)
    ang_r = row.astype(jnp.float32)[:, None] * inv_freq
    ang_c = col.astype(jnp.float32)[:, None] * inv_freq
    ang = jnp.concatenate([ang_r, ang_r, ang_c, ang_c], axis=-1)
    return jnp.cos(ang), jnp.sin(ang)


def apply_axial_rope(t, cos, sin):
    tq = t.reshape(t.shape[:-1] + (2, 2, HEAD_DIM // 4))
    rot = jnp.stack([-tq[..., 1, :], tq[..., 0, :]], axis=-2).reshape(t.shape)
    out = t.astype(jnp.float32) * cos[:, None, :] + rot.astype(jnp.float32) * sin[:, None, :]
    return out.astype(t.dtype)


def depthwise_conv(u, w):
    k = w.shape[0]
    pad = (k - 1) // 2
    return lax.conv_general_dilated(
        u, w[:, None, :].astype(u.dtype), window_strides=(1,), padding=((pad, pad),),
        dimension_numbers=("NWC", "WIO", "NWC"), feature_group_count=u.shape[-1])


def attend(q, k, v):
    s = jnp.einsum("bqkgd,bskd->bkgqs", q, k, preferred_element_type=jnp.float32) * HEAD_DIM ** -0.5
    p = jax.nn.softmax(s, axis=-1).astype(v.dtype)
    return jnp.einsum("bkgqs,bskd->bqkgd", p, v)


def latent_attention(q, k, v):
    b, s = q.shape[:2]
    nb = s // Q_BLOCK
    qb = q.reshape(b, nb, Q_BLOCK, N_KV_HEADS, GQA_GROUP, HEAD_DIM).transpose(1, 0, 2, 3, 4, 5)
    ob = lax.map(lambda qblk: attend(qblk, k, v), qb)
    return ob.transpose(1, 0, 2, 3, 4, 5).reshape(b, s, D_ATTN)


def context_attention(q, k, v):
    b, l = q.shape[:2]
    return attend(q.reshape(b, l, N_KV_HEADS, GQA_GROUP, HEAD_DIM), k, v).reshape(b, l, D_ATTN)


def short_conv_group(gate_b, gate_c, u, w):
    return gate_b * depthwise_conv(gate_c * u, w)


def conformer_group(a, g, w, b, ln_g, ln_b):
    u = a * jax.nn.sigmoid(g)
    u = depthwise_conv(u, w) + b
    return jax.nn.silu(layer_norm(u, ln_g, ln_b))


def project_groups(h, w_in, q_g, k_g, sconv_w, dw_w, dw_b, ln_g, ln_b):
    p = h @ w_in
    q, k, v, gb, gc, u, ca, cg = jnp.split(p, IN_SPLITS, axis=-1)
    q = rms_norm(heads(q, N_HEADS), q_g)
    k = rms_norm(heads(k, N_KV_HEADS), k_g)
    v = heads(v, N_KV_HEADS)
    y_sconv = short_conv_group(gb, gc, u, sconv_w)
    y_conf = conformer_group(ca, cg, dw_w, dw_b, ln_g, ln_b)
    return q, k, v, y_sconv, y_conf


def merge_groups(y_attn, y_sconv, y_conf, g, w_out):
    y = jnp.concatenate([
        rms_norm(y_attn, g[:D_ATTN]),
        rms_norm(y_sconv, g[D_ATTN:D_ATTN + D_SCONV]),
        rms_norm(y_conf, g[D_ATTN + D_SCONV:]),
    ], axis=-1)
    return y @ w_out


def swiglu_expert(h, w_gate, w_up, w_down):
    return (jax.nn.silu(h @ w_gate) * (h @ w_up)) @ w_down


def hierarchical_moe(h, wg, bg, we, be, w_gate, w_up, w_down):
    n_tok, d = h.shape
    g_prob = jax.nn.softmax((h @ wg).astype(jnp.float32) + bg, axis=-1)
    g_top, g_idx = lax.top_k(g_prob, 1)
    e_logits = ((h @ we).astype(jnp.float32) + be).reshape(n_tok, N_GROUPS, EXPERTS_PER_GROUP)
    e_logits = jnp.take_along_axis(e_logits, g_idx[:, :, None], axis=1)[:, 0]
    e_top, e_idx = lax.top_k(jax.nn.softmax(e_logits, axis=-1), TOP_K)
    weight = (g_top * e_top / jnp.sum(e_top, axis=-1, keepdims=True)).reshape(-1)
    expert = (g_idx * EXPERTS_PER_GROUP + e_idx).reshape(-1)
    token = jnp.repeat(jnp.arange(n_tok, dtype=jnp.int32), TOP_K)
    n_assign = expert.shape[0]
    order = jnp.argsort(expert)
    e_sorted = expert[order]
    counts = jnp.bincount(expert, length=N_EXPERTS)
    starts = jnp.cumsum(counts) - counts
    padded = (counts + EXPERT_BLOCK - 1) // EXPERT_BLOCK * EXPERT_BLOCK
    pad_end = jnp.cumsum(padded)
    dest = (pad_end - padded)[e_sorted] + jnp.arange(n_assign, dtype=jnp.int32) - starts[e_sorted]
    n_blocks = -(-n_assign // EXPERT_BLOCK) + N_EXPERTS
    n_rows = n_blocks * EXPERT_BLOCK
    row_tok = jnp.full((n_rows,), n_tok, jnp.int32).at[dest].set(token[order])
    row_w = jnp.zeros((n_rows,), jnp.float32).at[dest].set(weight[order])
    block_expert = jnp.minimum(
        jnp.searchsorted(pad_end, jnp.arange(n_blocks, dtype=jnp.int32) * EXPERT_BLOCK, side="right"),
        N_EXPERTS - 1)
    h_pad = jnp.concatenate([h, jnp.zeros((1, d), h.dtype)], axis=0)

    def run_block(args):
        tok_blk, w_blk, e = args
        y = swiglu_expert(h_pad[tok_blk], w_gate[e], w_up[e], w_down[e])
        return y * w_blk[:, None].astype(y.dtype)

    y = lax.map(run_block, (row_tok.reshape(n_blocks, EXPERT_BLOCK),
                            row_w.reshape(n_blocks, EXPERT_BLOCK), block_expert))
    out = jnp.zeros((n_tok + 1, d), h.dtype).at[row_tok].add(y.reshape(n_rows, d))
    return out[:n_tok]


def setup_inputs(seed: int = 0) -> dict:
    key = jax.random.key(seed)
    ks = jax.random.split(key, 26)
    f32 = jnp.float32

    def nrm(k, shape, s):
        return jax.random.normal(k, shape, f32) * s

    return {
        "x": nrm(ks[0], (BATCH, SEQ, D_MODEL), 1.0),
        "c": nrm(ks[1], (BATCH, D_MODEL), 1.0),
        "ctx": nrm(ks[2], (BATCH, CTX_LEN, D_MODEL), 1.0),
        "c_ctx": nrm(ks[3], (D_MODEL,), 1.0),
        "w_ada": nrm(ks[4], (DEPTH, D_MODEL, N_MOD * D_MODEL), 0.5 * D_MODEL ** -0.5),
        "b_ada": nrm(ks[5], (DEPTH, N_MOD * D_MODEL), 0.02),
        "norm1_g": 1 + nrm(ks[6], (DEPTH, D_MODEL), 0.02),
        "w_in": nrm(ks[7], (DEPTH, D_MODEL, D_IN_PROJ), D_MODEL ** -0.5),
        "q_norm_g": 1 + nrm(ks[8], (DEPTH, HEAD_DIM), 0.02),
        "k_norm_g": 1 + nrm(ks[9], (DEPTH, HEAD_DIM), 0.02),
        "sconv_w": nrm(ks[10], (DEPTH, SCONV_WIDTH, D_SCONV), SCONV_WIDTH ** -0.5),
        "conf_dw_w": nrm(ks[11], (DEPTH, CONF_WIDTH, D_CONF), CONF_WIDTH ** -0.5),
        "conf_dw_b": nrm(ks[12], (DEPTH, D_CONF), 0.02),
        "conf_ln_g": 1 + nrm(ks[13], (DEPTH, D_CONF), 0.02),
        "conf_ln_b": nrm(ks[14], (DEPTH, D_CONF), 0.02),
        "grp_norm_g": 1 + nrm(ks[15], (DEPTH, D_MIX), 0.02),
        "w_out": nrm(ks[16], (DEPTH, D_MIX, D_MODEL), D_MIX ** -0.5),
        "norm2_g": 1 + nrm(ks[17], (DEPTH, D_MODEL), 0.02),
        "router_g_w": nrm(ks[18], (DEPTH, D_MODEL, N_GROUPS), D_MODEL ** -0.5),
        "router_g_b": nrm(ks[19], (DEPTH, N_GROUPS), 0.01),
        "router_e_w": nrm(ks[20], (DEPTH, D_MODEL, N_EXPERTS), D_MODEL ** -0.5),
        "router_e_b": nrm(ks[21], (DEPTH, N_EXPERTS), 0.01),
        "exp_w_gate": nrm(ks[22], (DEPTH, N_EXPERTS, D_MODEL, D_EXPERT), D_MODEL ** -0.5),
        "exp_w_up": nrm(ks[23], (DEPTH, N_EXPERTS, D_MODEL, D_EXPERT), D_MODEL ** -0.5),
        "exp_w_down": nrm(ks[24], (DEPTH, N_EXPERTS, D_EXPERT, D_MODEL), D_EXPERT ** -0.5),
        "final_g": 1 + nrm(ks[25], (D_MODEL,), 0.02),
    }


def reference(x, c, ctx, c_ctx, w_ada, b_ada, norm1_g, w_in, q_norm_g, k_norm_g, sconv_w,
              conf_dw_w, conf_dw_b, conf_ln_g, conf_ln_b, grp_norm_g, w_out, norm2_g,
              router_g_w, router_g_b, router_e_w, router_e_b, exp_w_gate, exp_w_up, exp_w_down,
              final_g):
    b, s, d = x.shape
    l = ctx.shape[1]
    rows = s // GRID_W
    cos, sin = axial_rope_tables(rows)
    c_act = jax.nn.silu(c)
    c_ctx_act = jax.nn.silu(c_ctx)
    for i in range(DEPTH):
        last = i == DEPTH - 1
        mod = (c_act @ w_ada[i] + b_ada[i])[:, None, :]
        sh1, sc1, gt1, sh2, sc2, gt2 = jnp.split(mod, N_MOD, axis=-1)
        mod_c = c_ctx_act @ w_ada[i] + b_ada[i]
        csh1, csc1, cgt1, csh2, csc2, cgt2 = jnp.split(mod_c, N_MOD, axis=-1)

        h_ctx = modulate(rms_norm(ctx, norm1_g[i]), csh1, csc1)
        if last:
            k_c, v_c = jnp.split(h_ctx @ w_in[i][:, D_ATTN:D_ATTN + 2 * D_KV], 2, axis=-1)
            k_c = rms_norm(heads(k_c, N_KV_HEADS), k_norm_g[i])
            v_c = heads(v_c, N_KV_HEADS)
        else:
            q_c, k_c, v_c, ysc_c, ycf_c = project_groups(
                h_ctx, w_in[i], q_norm_g[i], k_norm_g[i], sconv_w[i], conf_dw_w[i], conf_dw_b[i],
                conf_ln_g[i], conf_ln_b[i])
            mix_c = merge_groups(context_attention(q_c, k_c, v_c), ysc_c, ycf_c, grp_norm_g[i], w_out[i])
            ctx = ctx + cgt1 * mix_c

        h_lat = modulate(rms_norm(x, norm1_g[i]), sh1, sc1)
        q_l, k_l, v_l, ysc_l, ycf_l = project_groups(
            h_lat, w_in[i], q_norm_g[i], k_norm_g[i], sconv_w[i], conf_dw_w[i], conf_dw_b[i],
            conf_ln_g[i], conf_ln_b[i])
        q_l = apply_axial_rope(q_l, cos, sin)
        k_l = apply_axial_rope(k_l, cos, sin)
        att_l = latent_attention(q_l, jnp.concatenate([k_l, k_c], axis=1),
                                 jnp.concatenate([v_l, v_c], axis=1))
        x = x + gt1 * merge_groups(att_l, ysc_l, ycf_l, grp_norm_g[i], w_out[i])

        h2_lat = modulate(rms_norm(x, norm2_g[i]), sh2, sc2).reshape(b * s, d)
        if last:
            f = hierarchical_moe(h2_lat, router_g_w[i], router_g_b[i], router_e_w[i], router_e_b[i],
                                 exp_w_gate[i], exp_w_up[i], exp_w_down[i])
            x = x + gt2 * f.reshape(b, s, d)
        else:
            h2_ctx = modulate(rms_norm(ctx, norm2_g[i]), csh2, csc2).reshape(b * l, d)
            f = hierarchical_moe(jnp.concatenate([h2_lat, h2_ctx], axis=0), router_g_w[i], router_g_b[i],
                                 router_e_w[i], router_e_b[i], exp_w_gate[i], exp_w_up[i], exp_w_down[i])
            x = x + gt2 * f[: b * s].reshape(b, s, d)
            ctx = ctx + cgt2 * f[b * s:].reshape(b, l, d)
    return rms_norm(x, final_g)
```

```python
import numpy as np
from contextlib import ExitStack
from functools import partial as P
import concourse.bass as bass
import concourse.mybir as mybir
from concourse.bass_utils import run_bass_kernel_spmd

F32 = mybir.dt.float32; BF16 = mybir.dt.bfloat16; U32 = mybir.dt.uint32; I32 = mybir.dt.int32
AF = mybir.ActivationFunctionType; ALU = mybir.AluOpType; AX = mybir.AxisListType
D = 2048; KC = 16; HD = 128; NH = 8; NKV = 2; NE = 32; DE = 512; EPS = 1e-6
GRID_W = 64
RELAX = True


class Tok:
    __slots__ = ("name", "w", "r", "rd", "hold")
    def __init__(s, name): s.name = name; s.w = None; s.r = {}; s.rd = []; s.hold = 0


class Op:
    __slots__ = ("eng", "fn", "deps", "signal", "sig", "dma", "key", "val")


class Sch:
    def __init__(s, nc, es):
        s.nc = nc; s.es = es; s.ops = []; s.cum = {}
        s.last = {}; s.lastdma = {}; s.pending = {}

    def barrier(s):
        b = set(s.last.values()) | set(s.lastdma.values())
        for e in ("pe", "act", "dve", "pool", "sp"):
            s.pending.setdefault(e, set()).update(b)

    def add(s, eng, fn, r=(), w=(), dma=False, key=None):
        op = Op(); op.eng = eng; op.fn = fn; op.dma = dma; op.signal = False; op.key = key; op.sig = 0; op.val = 0
        deps = set(); raw = set()
        for t in r:
            if t.w is not None: deps.add(t.w); raw.add(t.w)
        for t in w:
            if t.w is not None: deps.add(t.w)
            deps.update(t.r.values()); deps.update(t.rd)
        if eng in s.pending:
            pb = s.pending.pop(eng); deps.update(pb); raw.update(pb)
        fd = []
        for p in deps:
            if p is op: continue
            if (not p.dma) and (not dma) and p.eng == eng:
                if eng == "pe" or (RELAX and p not in raw): continue
            if not p.dma: p.signal = True
            fd.append(p)
        op.deps = fd
        if dma:
            assert key is not None
            s.cum[key] = s.cum.get(key, 0) + 16; op.val = s.cum[key]
        for t in r:
            if dma: t.rd.append(op)
            else: t.r[eng] = op
        for t in w:
            t.w = op; t.r = {}; t.rd = []
        if dma: s.lastdma[key] = op
        else: s.last[eng] = op
        s.ops.append(op)
        return op

    def emit(s):
        nc = s.nc
        eo = {"pe": nc.tensor, "act": nc.scalar, "dve": nc.vector, "pool": nc.gpsimd, "sp": nc.sync}
        esem = {e: s.es.enter_context(nc.semaphore("e_" + e)) for e in ("pe", "act", "dve", "pool")}
        dsem = {k: s.es.enter_context(nc.semaphore("d_%d" % i)) for i, k in enumerate(s.cum)}
        cnt = {e: 0 for e in esem}
        waited = {}
        for op in s.ops:
            need = {}
            for p in op.deps:
                if p.dma: k = ("d", p.key); v = p.val
                else: k = ("e", p.eng); v = p.sig
                if v > need.get(k, 0): need[k] = v
            for k, v in need.items():
                wk = (op.eng, k)
                if v > waited.get(wk, 0):
                    waited[wk] = v
                    eo[op.eng].wait_ge(dsem[k[1]] if k[0] == "d" else esem[k[1]], v)
            ins = op.fn()
            if op.dma:
                ins.then_inc(dsem[op.key], 16)
            elif op.signal:
                cnt[op.eng] += 1; op.sig = cnt[op.eng]
                ins.then_inc(esem[op.eng], 1)
        for k, v in s.cum.items():
            nc.sync.wait_ge(dsem[k], v)
        return len(dsem)


class Rot:
    def __init__(s, alloc, tk, name, shape, dt, n):
        s.bufs = [alloc("%s_%d" % (name, i), shape, dt) for i in range(n)]; s.toks = [tk(name) for _ in range(n)]; s.i = 0
    def next(s):
        i = s.i % len(s.bufs); s.i += 1
        return s.bufs[i], s.toks[i]


def pipeline(gens):
    active = []
    for g in gens:
        for a in list(active):
            try: next(a)
            except StopIteration: active.remove(a)
        active.append(g)
        try: next(g)
        except StopIteration: active.remove(g)
    while active:
        for a in list(active):
            try: next(a)
            except StopIteration: active.remove(a)


def build(S, L, NB, stop_after=None, dbg=False):
    NT = S + L; NTT = NT // 128; T = NB * NT
    assert S % 512 == 0 and L % 128 == 0 and L <= 512
    BLK = 256
    nc = bass.Bass("TRN2", target_bir_lowering=False)
    es = ExitStack()
    sch = Sch(nc, es)
    A = sch.add

    def dram(name, shape, dt=F32, kind="ExternalInput"):
        return nc.dram_tensor(name, shape, dt, kind=kind).ap()
    IK = "ExternalOutput" if dbg else "Internal"
    x_d = dram("x", [NB, S, D]); ctx_d = dram("ctx", [NB, L, D])
    cT_d = dram("cT", [128, KC * 3])
    wada_d = dram("w_ada", [2, D, 6 * D]); bada_d = dram("b_ada", [2, 1, 6 * D])
    NPV = 256
    pv_d = dram("pv", [2, 128, NPV])
    fg_d = dram("fg", [1, D])
    win_d = dram("w_in", [2, D, 4096]); wout_d = dram("w_out", [2, D, D])
    wr_d = dram("wr", [2, D, 36]); rb_d = dram("rb", [2, 1, 36])
    wg_d = dram("wg", [2, NE * 2 * 128, KC * 256]); wu_d = dram("wu", [2, NE * 2 * 128, KC * 256])
    wd_d = dram("wd", [2, NE * 128, 4 * D])
    cs_d = dram("cs", [2, 128, S])
    perm_d = dram("perm", [128, 128])
    UCd = dram("UCd", [NB, 4, 128, NT], F32, kind=IK)
    out_d = dram("out", [NB, S, D], kind="ExternalOutput")
    MOD = dram("MOD", [2, 3, 6 * D], kind=IK)
    QT = dram("QT", [NB, NH, 128, NT], BF16, kind=IK)
    KT = dram("KT", [NB, NKV, 128, NT], BF16, kind=IK)
    VV = dram("VV", [NB, NT, 256], BF16, kind=IK)
    YT = dram("YT", [NB, KC, 128, NT], F32, kind=IK)
    R = dram("R", [NB, NT, D], F32, kind=IK)
    H2 = dram("H2", [T, D], BF16, kind=IK)
    NBLK_MAX = (2 * T + BLK - 1) // BLK + NE
    XS = dram("XS", [NBLK_MAX * BLK, D], BF16, kind="Internal")
    YS = dram("YS", [NBLK_MAX * BLK, D], F32, kind="Internal")
    if dbg:
        dbg_d = dram("dbg", [NB, NT, D], kind="ExternalOutput")

    ucnt = [0]
    def uname(name):
        ucnt[0] += 1
        return "s%d_%s" % (ucnt[0], name)
    def sb(name, shape, dt=F32):
        return es.enter_context(nc.sbuf_tensor(uname(name), shape, dt))
    tokc = [0]
    def tk(name="t"):
        tokc[0] += 1
        return Tok("%s%d" % (name, tokc[0]))

    PS = [es.enter_context(nc.psum_tensor("ps%d" % i, [128, 512], F32)) for i in range(8)]
    PT = [tk("ps") for _ in range(8)]
    psrr = [0]
    def psnext():
        for _ in range(8):
            i = psrr[0] % 8; psrr[0] += 1
            t = PT[i]
            if getattr(t, "hold", 0) == 0 and (t.w is None or t.r or t.rd):
                return PS[i], t
        raise RuntimeError("PSUM: no free bank (too many live tiles in the pipeline)")

    ident = sb("ident", [128, 128]); t_ident = tk()
    identb = sb("identb", [128, 128], BF16); t_identb = tk()
    onesb = sb("onesb", [128, 128], BF16); t_onesb = tk()
    onesf = sb("onesf", [128, 128]); t_onesf = tk()
    perm = sb("perm", [128, 128]); t_perm = tk()
    ustr = sb("ustr", [128, 128], BF16); t_ustr = tk()
    iot = sb("iot", [128, 128]); t_iot = tk()
    pidx = sb("pidx", [128, 1]); t_pidx = tk()
    eps_t = sb("eps_t", [128, 1]); t_eps = tk()
    iot32 = sb("iot32", [128, 32]); t_iot32 = tk()
    A("pool", P(nc.gpsimd.iota, iot[:], pattern=[[1, 128]], base=0, channel_multiplier=0, allow_small_or_imprecise_dtypes=True), w=[t_iot])
    A("pool", P(nc.gpsimd.iota, pidx[:], pattern=[[0, 1]], base=0, channel_multiplier=1, allow_small_or_imprecise_dtypes=True), w=[t_pidx])
    A("dve", P(nc.vector.memset, eps_t[:], EPS), w=[t_eps])
    A("dve", P(nc.vector.memset, onesb[:], 1.0), w=[t_onesb])
    A("dve", P(nc.vector.memset, onesf[:], 1.0), w=[t_onesf])
    A("dve", P(nc.vector.tensor_copy, out=iot32[:], in_=iot[:, 0:32]), r=[t_iot], w=[t_iot32])
    A("dve", P(nc.vector.tensor_scalar, out=ident[:], in0=iot[:], scalar1=pidx[:, 0:1], scalar2=None, op0=ALU.is_equal), r=[t_iot, t_pidx], w=[t_ident])
    A("dve", P(nc.vector.tensor_copy, out=identb[:], in_=ident[:]), r=[t_ident], w=[t_identb])
    A("dve", P(nc.vector.tensor_scalar, out=ustr[:], in0=iot[:], scalar1=pidx[:, 0:1], scalar2=None, op0=ALU.is_gt), r=[t_iot, t_pidx], w=[t_ustr])
    A("sp", P(nc.sync.dma_start, out=perm[:], in_=perm_d), w=[t_perm], dma=True, key="perm")

    cosT = sb("cosT", [128, S]); sinT = sb("sinT", [128, S]); t_cs = tk()
    A("sp", P(nc.sync.dma_start, out=cosT[:], in_=cs_d[0]), w=[t_cs], dma=True, key="cs")
    A("sp", P(nc.sync.dma_start, out=sinT[:], in_=cs_d[1]), w=[t_cs], dma=True, key="cs")
    pv = [sb("pv%d" % l, [128, NPV]) for l in range(2)]; t_pv = [tk() for _ in range(2)]
    for l in range(2):
        A("sp", (P(nc.sync.dma_start, out=pv[l][:], in_=pv_d[l])), w=[t_pv[l]], dma=True, key="pv%d" % l)
    PV_G1, PV_G2, PV_GG, PV_QG, PV_KG, PV_SW, PV_CW, PV_CB, PV_LG, PV_LB = 0, 16, 32, 48, 49, 50, 62, 186, 190, 194

    csil = sb("csil", [128, KC * 3]); t_csil = tk()
    A("sp", P(nc.sync.dma_start, out=csil[:], in_=cT_d), w=[t_csil], dma=True, key="csil")
    A("act", P(nc.scalar.activation, out=csil[:], in_=csil[:], func=AF.Silu), r=[t_csil], w=[t_csil])
    modT = [sb("modT%d" % l, [128, 96 * 3]) for l in range(2)]; t_modT = [tk() for _ in range(2)]
    with ExitStack() as ph:
        sch.barrier()
        def psb(name, shape, dt=F32):
            return ph.enter_context(nc.sbuf_tensor(uname(name), shape, dt))
        wa = [psb("wa%d" % i, [128, KC, 512]) for i in range(2)]; t_wa = [tk() for _ in range(2)]
        mrow = psb("mrow", [3, 6 * D]); t_mrow = tk()
        brows = [psb("brow%d" % i, [3, 512]) for i in range(2)]; t_brows = [tk() for _ in range(2)]
        t_MODs = [tk("MOD") for _ in range(2)]
        for l in range(2):
            for nb in range(24):
                i = nb % 2
                brow = brows[i]; t_brow = t_brows[i]
                A("sp", (P(nc.sync.dma_start, out=brow[:], in_=bada_d[l, 0, nb * 512:(nb + 1) * 512].partition_broadcast(3))), w=[t_brow], dma=True, key="brow%d" % i)
                A("sp", (P(nc.sync.dma_start, out=wa[i][:], in_=wada_d[l, :, nb * 512:(nb + 1) * 512].rearrange("(c p) n -> p c n", p=128))), w=[t_wa[i]], dma=True, key="wa%d" % i)
                pt, ptk = psnext()
                for c in range(KC):
                    A("pe", (P(nc.tensor.matmul, pt[0:3, :], lhsT=csil[:, c * 3:(c + 1) * 3], rhs=wa[i][:, c, :], start=(c == 0), stop=(c == KC - 1))), r=[t_csil, t_wa[i]], w=[ptk])
                A("dve", (P(nc.vector.tensor_tensor, out=mrow[:, nb * 512:(nb + 1) * 512], in0=pt[0:3, :], in1=brow[:, :], op=ALU.add)), r=[ptk, t_brow], w=[t_mrow])
            A("sp", (P(nc.sync.dma_start, out=MOD[l], in_=mrow[:])), r=[t_mrow], w=[t_MODs[l]], dma=True, key="mrow")
            for j0 in range(0, 96, 32):
                pt, ptk = psnext()
                for j in range(j0, j0 + 32):
                    A("pe", (P(nc.tensor.transpose, pt[:, (j - j0) * 3:(j - j0 + 1) * 3], mrow[0:3, j * 128:(j + 1) * 128], ident[0:3, 0:3])), r=[t_mrow, t_ident], w=[ptk])
                A("dve", (P(nc.vector.tensor_copy, out=modT[l][:, j0 * 3:(j0 + 32) * 3], in_=pt[:, 0:96])), r=[ptk], w=[t_modT[l]])
    t_MODd = tk("MOD")

    def modv(l, sec, r3):
        return modT[l][:, sec * 48:(sec + 1) * 48].rearrange("p (c r) -> p c r", r=3)[:, :, r3]

    Avec = [[sb("Av%d_%d" % (l, k), [128, 3 * KC]) for k in range(2)] for l in range(2)]
    t_Av = [tk() for _ in range(2)]
    for l in range(2):
        for k, (sec, gcol) in enumerate(((1, PV_G1), (4, PV_G2))):
            for r3 in range(3):
                A("dve", (P(nc.vector.scalar_tensor_tensor,
                    out=Avec[l][k][:, r3 * KC:(r3 + 1) * KC], in0=modv(l, sec, r3), scalar=1.0, in1=pv[l][:, gcol:gcol + KC], op0=ALU.add, op1=ALU.mult)),
                  r=[t_modT[l], t_pv[l]], w=[t_Av[l]])

    state = {"rtok": [[tk("R") for _ in range(NTT)] for _ in range(NB)]}

    def resid_src(l, b, tt):
        if l == 0:
            if tt < S // 128: return x_d[b, tt * 128:(tt + 1) * 128, :]
            return ctx_d[b, tt * 128 - S:(tt + 1) * 128 - S, :]
        return R[b, tt * 128:(tt + 1) * 128, :]

    tgs = [(g * 512, 512) for g in range(S // 512)] + [(S, L)]

    for l in range(2):
        last = (l == 1)
        t_QT = [tk("QT") for _ in range(NB)]; t_KT = [tk("KT") for _ in range(NB)]; t_VV = [tk("VV") for _ in range(NB)]
        t_YT = [[tk("YT") for _ in range(KC)] for _ in range(NB)]
        for b in range(NB):
            with ExitStack() as ph:
                sch.barrier()
                def psb(name, shape, dt=F32):
                    return ph.enter_context(nc.sbuf_tensor(uname(name), shape, dt))
                hT = psb("hT", [128, KC, NT], BF16); t_hT = [[tk("hTlo"), tk("hThi")] for _ in range(NTT)]
                with ExitStack() as ph1:
                    sch.barrier()
                    def psb1(name, shape, dt=F32):
                        return ph1.enter_context(nc.sbuf_tensor(uname(name), shape, dt))
                    xtR = Rot(psb1, tk, "xt", [128, D], F32, 4)
                    xnR = Rot(psb1, tk, "xn", [128, D], F32, 3)
                    junkR = Rot(psb1, tk, "junk", [128, D], BF16, 2)
                    stR = Rot(psb1, tk, "st", [128, 4], F32, 6)

                    def b1_item(tt):
                        r3 = b if tt < S // 128 else 2
                        xt_, t_xt_ = xtR.next(); st, t_st = stR.next()
                        A("sp", (P(nc.sync.dma_start, out=xt_[:], in_=resid_src(l, b, tt))), r=[state["rtok"][b][tt]], w=[t_xt_], dma=True, key="xt%d" % (xtR.i % 4))
                        yield
                        junk, t_junk = junkR.next()
                        A("act", (P(nc.scalar.activation, out=junk[:], in_=xt_[:], func=AF.Square, accum_out=st[:, 0:1])), r=[t_xt_], w=[t_junk, t_st])
                        A("act", (P(nc.scalar.activation, out=st[:, 1:2], in_=st[:, 0:1], func=AF.Sqrt, scale=1.0 / D, bias=eps_t[:, 0:1])), r=[t_st, t_eps], w=[t_st])
                        A("dve", (P(nc.vector.reciprocal, out=st[:, 2:3], in_=st[:, 1:2])), r=[t_st], w=[t_st])
                        yield
                        xn_, t_xn_ = xnR.next()
                        A("act", (P(nc.scalar.activation, out=xn_[:], in_=xt_[:], func=AF.Identity, scale=st[:, 2:3])), r=[t_xt_, t_st], w=[t_xn_])
                        yield
                        pts = []
                        for c0 in range(0, KC, 4):
                            pt, ptk = psnext(); pts.append((pt, ptk))
                            for c in range(c0, c0 + 4):
                                A("pe", (P(nc.tensor.transpose, pt[:, (c - c0) * 128:(c - c0 + 1) * 128], xn_[:, c * 128:(c + 1) * 128], ident[:])), r=[t_xn_, t_ident], w=[ptk])
                        yield
                        for c0 in range(0, KC, 4):
                            pt, ptk = pts[c0 // 4]
                            for c in range(c0, c0 + 4):
                                sc_ap = Avec[l][0][:, r3 * KC + c: r3 * KC + c + 1]
                                bi_ap = modT[l][:, (0 * 16 + c) * 3 + r3:(0 * 16 + c) * 3 + r3 + 1]
                                if c < 8:
                                    A("dve", (P(nc.vector.tensor_scalar, out=hT[:, c, tt * 128:(tt + 1) * 128], in0=pt[:, (c - c0) * 128:(c - c0 + 1) * 128], scalar1=sc_ap, scalar2=bi_ap, op0=ALU.mult, op1=ALU.add)),
                                      r=[ptk, t_Av[l], t_modT[l]], w=[t_hT[tt][c // 8]])
                                else:
                                    A("act", (P(nc.scalar.activation, out=hT[:, c, tt * 128:(tt + 1) * 128], in_=pt[:, (c - c0) * 128:(c - c0 + 1) * 128], func=AF.Identity, scale=sc_ap, bias=bi_ap)),
                                      r=[ptk, t_Av[l], t_modT[l]], w=[t_hT[tt][c // 8]])
                    pipeline(b1_item(tt) for tt in range(NTT))
                sch.barrier()
                wt = [psb("wt%d" % i, [128, KC, 512], BF16) for i in range(2)]; t_wt = [tk() for _ in range(2)]
                SEQW = 16 + S + 16 + L + 16
                OFFC = 16 + S + 16
                def seqoff(t0):
                    return 16 + t0 if t0 < S else OFFC + (t0 - S)
                sqR = Rot(psb, tk, "sq", [128, 512], BF16, 2)
                rsR = Rot(psb, tk, "rs", [128, 512], F32, 2)
                qnR = Rot(psb, tk, "qn", [128, 512], F32, 2)
                t1R = Rot(psb, tk, "t1", [128, 512], F32, 2)
                t2R = Rot(psb, tk, "t2", [128, 512], F32, 2)
                qoR = Rot(psb, tk, "qo", [128, 512], BF16, 3)
                vo = [psb("vo%d" % i, [128, 256], BF16) for i in range(2)]; t_vo = [tk() for _ in range(2)]
                wgi = [0]
                def load_w(col0, ncol):
                    i = wgi[0] % 2; wgi[0] += 1
                    A("pool", (P(nc.gpsimd.dma_start, out=wt[i][:, :, 0:ncol], in_=win_d[l, :, col0:col0 + ncol].rearrange("(c p) n -> p c n", p=128))), w=[t_wt[i]], dma=True, key="wt%d" % i)
                    return i

                def proj(i, j, t0, n, pt, ptk):
                    for c in range(KC):
                        tts = [t_hT[t][c // 8] for t in range(t0 // 128, (t0 + n) // 128)]
                        A("pe", (P(nc.tensor.matmul, pt[:, 0:n], lhsT=wt[i][:, c, j * 128:(j + 1) * 128], rhs=hT[:, c, t0:t0 + n], start=(c == 0), stop=(c == KC - 1))), r=[t_wt[i]] + tts, w=[ptk])

                def qk_item(i, j, gcol, dst, t_dst, t0, n):
                    pt, ptk = psnext(); ptk.hold = 1
                    proj(i, j, t0, n, pt, ptk)
                    sq, t_sq = sqR.next()
                    A("act", (P(nc.scalar.activation, out=sq[:, 0:n], in_=pt[:, 0:n], func=AF.Square)), r=[ptk], w=[t_sq])
                    yield
                    p2, p2k = psnext()
                    rs, t_rs = rsR.next()
                    A("pe", (P(nc.tensor.matmul, p2[:, 0:n], lhsT=onesb[:], rhs=sq[:, 0:n], start=True, stop=True)), r=[t_sq, t_onesb], w=[p2k])
                    A("act", (P(nc.scalar.activation, out=rs[:, 0:n], in_=p2[:, 0:n], func=AF.Sqrt, scale=1.0 / HD, bias=eps_t[:, 0:1])), r=[p2k, t_eps], w=[t_rs])
                    A("dve", (P(nc.vector.reciprocal, out=rs[:, 0:n], in_=rs[:, 0:n])), r=[t_rs], w=[t_rs])
                    qo_, t_qo_ = qoR.next(); qkey = "qo%d" % (qoR.i % 3)
                    if t0 >= S:
                        A("dve", (P(nc.vector.scalar_tensor_tensor, out=qo_[:, 0:n], in0=pt[:, 0:n], scalar=pv[l][:, gcol:gcol + 1], in1=rs[:, 0:n], op0=ALU.mult, op1=ALU.mult)), r=[ptk, t_rs, t_pv[l]], w=[t_qo_])
                        ptk.hold = 0
                    else:
                        qn, t_qn = qnR.next(); t1, t_t1 = t1R.next(); t2, t_t2 = t2R.next()
                        A("dve", (P(nc.vector.scalar_tensor_tensor, out=qn[:, 0:n], in0=pt[:, 0:n], scalar=pv[l][:, gcol:gcol + 1], in1=rs[:, 0:n], op0=ALU.mult, op1=ALU.mult)), r=[ptk, t_rs, t_pv[l]], w=[t_qn])
                        ptk.hold = 0
                        A("pool", (P(nc.gpsimd.tensor_tensor, out=t1[:, 0:n], in0=qn[:, 0:n], in1=cosT[:, t0:t0 + n], op=ALU.mult)), r=[t_qn, t_cs], w=[t_t1])
                        yield
                        p3, p3k = psnext()
                        A("pe", (P(nc.tensor.matmul, p3[:, 0:n], lhsT=perm[:], rhs=qn[:, 0:n], start=True, stop=True)), r=[t_qn, t_perm], w=[p3k])
                        A("dve", (P(nc.vector.tensor_tensor, out=t2[:, 0:n], in0=p3[:, 0:n], in1=sinT[:, t0:t0 + n], op=ALU.mult)), r=[p3k, t_cs], w=[t_t2])
                        A("dve", (P(nc.vector.tensor_tensor, out=qo_[:, 0:n], in0=t1[:, 0:n], in1=t2[:, 0:n], op=ALU.add)), r=[t_t1, t_t2], w=[t_qo_])
                    A("sp", (P(nc.sync.dma_start, out=dst[:, t0:t0 + n], in_=qo_[:, 0:n])), r=[t_qo_], w=[t_dst], dma=True, key=qkey)

                def qk_chunk(i, j, gcol, dst, t_dst):
                    return [qk_item(i, j, gcol, dst, t_dst, t0, n) for (t0, n) in tgs]

                for g in range(2):
                    i = load_w(g * 512, 512)
                    items = []
                    for j in range(4):
                        items += qk_chunk(i, j, PV_QG, QT[b, g * 4 + j], t_QT[b])
                    pipeline(items)
                i = load_w(1024, 512)
                items = []
                for j in range(2):
                    items += qk_chunk(i, j, PV_KG, KT[b, j], t_KT[b])
                pipeline(items)
                for tt in range(NTT):
                    pt, ptk = psnext()
                    for c in range(KC):
                        A("pe", (P(nc.tensor.matmul, pt[:, 0:256], lhsT=hT[:, c, tt * 128:(tt + 1) * 128], rhs=wt[i][:, c, 256:512], start=(c == 0), stop=(c == KC - 1))), r=[t_wt[i], t_hT[tt][c // 8]], w=[ptk])
                    vi = tt % 2
                    A("act", (P(nc.scalar.activation, func=AF.Copy, out=vo[vi][:], in_=pt[:, 0:256])), r=[ptk], w=[t_vo[vi]])
                    A("sp", (P(nc.sync.dma_start, out=VV[b, tt * 128:(tt + 1) * 128, :], in_=vo[vi][:])), r=[t_vo[vi]], w=[t_VV[b]], dma=True, key="vo%d" % vi)

                seqA = psb("seqA", [128, SEQW], BF16); t_seqA = tk()
                gbuf = psb("gbuf", [128, NT]); t_gbuf = tk()
                tmpfR = Rot(psb, tk, "tmpf", [128, 512], F32, 2)
                dg = psb("dg", [128, 31, 128], BF16); t_dg = tk()
                yo = [psb("yo%d" % i2, [128, 512]) for i2 in range(2)]; t_yo = [tk() for _ in range(2)]
                t_UC = [tk("UC") for _ in range(4)]
                A("dve", P(nc.vector.memset, seqA[:], 0.0), w=[t_seqA])
                yoc = [0]

                def build_diag(dgt, t_dgt, ntap, col0, stride, j):
                    for k in range(ntap):
                        A("dve", (P(nc.vector.tensor_scalar, out=dgt[:, k, :], in0=ident[:], scalar1=pv[l][:, col0 + k * stride + j: col0 + k * stride + j + 1], scalar2=None, op0=ALU.mult)), r=[t_ident, t_pv[l]], w=[t_dgt])

                def dwconv(dgt, t_dgt, seq, t_seq, ntap, t0, n, pt, ptk):
                    half = (ntap - 1) // 2
                    o = seqoff(t0)
                    for k in range(ntap):
                        A("pe", (P(nc.tensor.matmul, pt[:, 0:n], lhsT=dgt[:, k, :], rhs=seq[:, o + k - half: o + k - half + n], start=(k == 0), stop=(k == ntap - 1))), r=[t_dgt, t_seq], w=[ptk])

                for j in range(4):
                    i = load_w(1536 + j * 384, 384)
                    build_diag(dg, t_dg, 3, PV_SW, 4, j)
                    for (t0, n) in tgs:
                        pb, pbk = psnext(); proj(i, 0, t0, n, pb, pbk)
                        pc, pck = psnext(); proj(i, 1, t0, n, pc, pck)
                        pu, puk = psnext(); proj(i, 2, t0, n, pu, puk)
                        A("act", (P(nc.scalar.activation, func=AF.Copy, out=gbuf[:, t0:t0 + n], in_=pb[:, 0:n])), r=[pbk], w=[t_gbuf])
                        tmpf, t_tmpf = tmpfR.next()
                        A("act", (P(nc.scalar.activation, func=AF.Copy, out=tmpf[:, 0:n], in_=pc[:, 0:n])), r=[pck], w=[t_tmpf])
                        A("dve", (P(nc.vector.tensor_tensor, out=seqA[:, seqoff(t0):seqoff(t0) + n], in0=tmpf[:, 0:n], in1=pu[:, 0:n], op=ALU.mult)), r=[t_tmpf, puk], w=[t_seqA])
                    for (t0, n) in tgs:
                        pt, ptk = psnext()
                        dwconv(dg, t_dg, seqA, t_seqA, 3, t0, n, pt, ptk)
                        yi = yoc[0] % 2; yoc[0] += 1
                        A("dve", (P(nc.vector.tensor_tensor, out=yo[yi][:, 0:n], in0=pt[:, 0:n], in1=gbuf[:, t0:t0 + n], op=ALU.mult)), r=[ptk, t_gbuf], w=[t_yo[yi]])
                        A("sp", (P(nc.sync.dma_start, out=YT[b, 8 + j, :, t0:t0 + n], in_=yo[yi][:, 0:n])), r=[t_yo[yi]], w=[t_YT[b][8 + j]], dma=True, key="yo%d" % yi)
                for g in range(2):
                    i = load_w(3072 + g * 512, 512)
                    for jj in range(2):
                        j = 2 * g + jj
                        build_diag(dg, t_dg, 31, PV_CW, 4, j)
                        for (t0, n) in tgs:
                            pa, pak = psnext(); proj(i, 2 * jj, t0, n, pa, pak)
                            pg, pgk = psnext(); proj(i, 2 * jj + 1, t0, n, pg, pgk)
                            tmpf, t_tmpf = tmpfR.next()
                            A("act", (P(nc.scalar.activation, out=tmpf[:, 0:n], in_=pg[:, 0:n], func=AF.Sigmoid)), r=[pgk], w=[t_tmpf])
                            A("dve", (P(nc.vector.tensor_tensor, out=seqA[:, seqoff(t0):seqoff(t0) + n], in0=tmpf[:, 0:n], in1=pa[:, 0:n], op=ALU.mult)), r=[t_tmpf, pak], w=[t_seqA])
                        for (t0, n) in tgs:
                            pt, ptk = psnext()
                            dwconv(dg, t_dg, seqA, t_seqA, 31, t0, n, pt, ptk)
                            yi = yoc[0] % 2; yoc[0] += 1
                            A("act", (P(nc.scalar.activation, out=yo[yi][:, 0:n], in_=pt[:, 0:n], func=AF.Identity, bias=pv[l][:, PV_CB + j:PV_CB + j + 1])), r=[ptk, t_pv[l]], w=[t_yo[yi]])
                            A("sp", (P(nc.sync.dma_start, out=UCd[b, j, :, t0:t0 + n], in_=yo[yi][:, 0:n])), r=[t_yo[yi]], w=[t_UC[j]], dma=True, key="yo%d" % yi)
            with ExitStack() as ph:
                sch.barrier()
                def psb(name, shape, dt=F32):
                    return ph.enter_context(nc.sbuf_tensor(uname(name), shape, dt))
                ucl = [psb("ucl%d" % i2, [128, 4, 512]) for i2 in range(2)]; t_ucl = [tk() for _ in range(2)]
                mean = psb("mean", [128, 512]); t_mean = tk()
                var = psb("var", [128, 512]); t_var = tk()
                tmpfR = Rot(psb, tk, "tmpf3", [128, 512], F32, 2)
                usqR = Rot(psb, tk, "usq", [128, 512], F32, 2)
                yo = [psb("yo3_%d" % i2, [128, 512]) for i2 in range(2)]; t_yo = [tk() for _ in range(2)]
                yoc = [0]
                for gi_, (t0, n) in enumerate(tgs):
                    ui = gi_ % 2; uc = ucl[ui]
                    for j in range(4):
                        A("sp", (P(nc.sync.dma_start, out=uc[:, j, 0:n], in_=UCd[b, j, :, t0:t0 + n])), r=[t_UC[j]], w=[t_ucl[ui]], dma=True, key="ucl%d" % ui)
                    p1, p1k = psnext(); p2, p2k = psnext()
                    for j in range(4):
                        A("pe", (P(nc.tensor.matmul, p1[:, 0:n], lhsT=onesf[:], rhs=uc[:, j, 0:n], start=(j == 0), stop=(j == 3))), r=[t_ucl[ui], t_onesf], w=[p1k])
                    for j in range(4):
                        usq, t_usq = usqR.next()
                        A("pool", (P(nc.gpsimd.tensor_tensor, out=usq[:, 0:n], in0=uc[:, j, 0:n], in1=uc[:, j, 0:n], op=ALU.mult)), r=[t_ucl[ui]], w=[t_usq])
                        A("pe", (P(nc.tensor.matmul, p2[:, 0:n], lhsT=onesf[:], rhs=usq[:, 0:n], start=(j == 0), stop=(j == 3))), r=[t_usq, t_onesf], w=[p2k])
                    A("act", (P(nc.scalar.activation, out=mean[:, 0:n], in_=p1[:, 0:n], func=AF.Copy, scale=1.0 / 512)), r=[p1k], w=[t_mean])
                    A("dve", (P(nc.vector.tensor_tensor, out=var[:, 0:n], in0=mean[:, 0:n], in1=mean[:, 0:n], op=ALU.mult)), r=[t_mean], w=[t_var])
                    A("dve", (P(nc.vector.scalar_tensor_tensor, out=var[:, 0:n], in0=p2[:, 0:n], scalar=1.0 / 512, in1=var[:, 0:n], op0=ALU.mult, op1=ALU.subtract)), r=[p2k, t_var], w=[t_var])
                    A("act", (P(nc.scalar.activation, out=var[:, 0:n], in_=var[:, 0:n], func=AF.Sqrt, bias=eps_t[:, 0:1])), r=[t_var, t_eps], w=[t_var])
                    A("dve", (P(nc.vector.reciprocal, out=var[:, 0:n], in_=var[:, 0:n])), r=[t_var], w=[t_var])
                    for j in range(4):
                        tmpf, t_tmpf = tmpfR.next()
                        A("dve", (P(nc.vector.tensor_tensor, out=tmpf[:, 0:n], in0=uc[:, j, 0:n], in1=mean[:, 0:n], op=ALU.subtract)), r=[t_ucl[ui], t_mean], w=[t_tmpf])
                        A("dve", (P(nc.vector.tensor_tensor, out=tmpf[:, 0:n], in0=tmpf[:, 0:n], in1=var[:, 0:n], op=ALU.mult)), r=[t_tmpf, t_var], w=[t_tmpf])
                        yi = yoc[0] % 2; yoc[0] += 1
                        A("act", (P(nc.scalar.activation, out=yo[yi][:, 0:n], in_=tmpf[:, 0:n], func=AF.Silu, scale=pv[l][:, PV_LG + j:PV_LG + j + 1], bias=pv[l][:, PV_LB + j:PV_LB + j + 1])), r=[t_tmpf, t_pv[l]], w=[t_yo[yi]])
                        A("sp", (P(nc.sync.dma_start, out=YT[b, 12 + j, :, t0:t0 + n], in_=yo[yi][:, 0:n])), r=[t_yo[yi]], w=[t_YT[b][12 + j]], dma=True, key="yo3_%d" % yi)

        SCALE = HD ** -0.5
        for b in range(NB):
            with ExitStack() as ph:
                sch.barrier()
                def psb(name, shape, dt=F32):
                    return ph.enter_context(nc.sbuf_tensor(uname(name), shape, dt))
                kt = [psb("kt%d" % i, [128, NT], BF16) for i in range(2)]; t_kt = [tk() for _ in range(2)]
                vt = [psb("vt%d" % i, [128, NTT, 128], BF16) for i in range(2)]; t_vt = [tk() for _ in range(2)]
                for kv in range(NKV):
                    A("sp", (P(nc.sync.dma_start, out=kt[kv][:], in_=KT[b, kv])), r=[t_KT[b]], w=[t_kt[kv]], dma=True, key="kt%d" % kv)
                    A("sp", (P(nc.sync.dma_start, out=vt[kv][:], in_=VV[b, :, kv * 128:(kv + 1) * 128].rearrange("(t p) d -> p t d", p=128))), r=[t_VV[b]], w=[t_vt[kv]], dma=True, key="vt%d" % kv)
                qc = [0]
                qgroups = [(g * 512, 512, 0, NTT) for g in range(S // 512)]
                if not last:
                    qgroups.append((S, L, S // 128, NTT))
                pTR = Rot(psb, tk, "pTr", [128, 512], BF16, 4)
                sbank = [0]

                rdenR = Rot(psb, tk, "rdenr", [128, 512], F32, 2)
                ooR = Rot(psb, tk, "oor", [128, 512], F32, 2)
                qtR = Rot(psb, tk, "qtr", [128, 512], BF16, 3)
                def allitems2():
                    for h in range(NH):
                        for (t0, n, k0, k1) in qgroups:
                            ab = (qc[0] % 2) * 2; qc[0] += 1
                            kv = h // (NH // NKV)
                            qt_, t_qt_ = qtR.next(); qkey = "qt%d" % (qtR.i % 3)
                            A("sp", (P(nc.sync.dma_start, out=qt_[:, 0:n], in_=QT[b, h, :, t0:t0 + n])), r=[t_QT[b]], w=[t_qt_], dma=True, key=qkey)
                            po, pok = PS[ab], PT[ab]; pd, pdk = PS[ab + 1], PT[ab + 1]
                            for kt_i in range(k0, k1):
                                yield kt_item2(po, pok, pd, pdk, kv, kt_i, qt_, t_qt_, n, k0, k1, h, t0)

                def kt_item2(po, pok, pd, pdk, kv, kt_i, qt_, t_qt_, n, k0, k1, h, t0):
                    si = 4 + (sbank[0] % 4); sbank[0] += 1
                    ps_, psk = PS[si], PT[si]
                    pT_, t_pT_ = pTR.next()
                    A("pe", (P(nc.tensor.matmul, ps_[:, 0:n], lhsT=kt[kv][:, kt_i * 128:(kt_i + 1) * 128], rhs=qt_[:, 0:n], start=True, stop=True)), r=[t_kt[kv], t_qt_], w=[psk])
                    A("act", (P(nc.scalar.activation, out=pT_[:, 0:n], in_=ps_[:, 0:n], func=AF.Exp, scale=SCALE)), r=[psk], w=[t_pT_])
                    yield
                    yield
                    A("pe", (P(nc.tensor.matmul, po[:, 0:n], lhsT=vt[kv][:, kt_i, :], rhs=pT_[:, 0:n], start=(kt_i == k0), stop=(kt_i == k1 - 1))), r=[t_vt[kv], t_pT_], w=[pok])
                    A("pe", (P(nc.tensor.matmul, pd[:, 0:n], lhsT=onesb[:], rhs=pT_[:, 0:n], start=(kt_i == k0), stop=(kt_i == k1 - 1))), r=[t_onesb, t_pT_], w=[pdk])
                    if kt_i == k1 - 1:
                        rden, t_rden = rdenR.next(); oo_, t_oo_ = ooR.next(); okey = "oo%d" % (ooR.i % 2)
                        A("dve", (P(nc.vector.reciprocal, out=rden[:, 0:n], in_=pd[:, 0:n])), r=[pdk], w=[t_rden])
                        A("dve", (P(nc.vector.tensor_tensor, out=oo_[:, 0:n], in0=po[:, 0:n], in1=rden[:, 0:n], op=ALU.mult)), r=[pok, t_rden], w=[t_oo_])
                        A("sp", (P(nc.sync.dma_start, out=YT[b, h, :, t0:t0 + n], in_=oo_[:, 0:n])), r=[t_oo_], w=[t_YT[b][h]], dma=True, key=okey)
                pipeline(allitems2())

        ntok_moe = NT if not last else S
        NTM = ntok_moe // 128
        TM = NB * ntok_moe
        NBLK = (2 * TM + BLK - 1) // BLK + NE
        t_H2 = tk("H2")
        with ExitStack() as phm:
            sch.barrier()
            def psbm(name, shape, dt=F32):
                return phm.enter_context(nc.sbuf_tensor(uname(name), shape, dt))
            RW = psbm("RW", [128, NB * NTM, 2]); t_RW = tk()
            M1 = psbm("M1", [128, NB * NTM, 32]); M2 = psbm("M2", [128, NB * NTM, 32]); t_M = tk()
            RK = psbm("RK", [128, NB * NTM, 2]); t_RK = tk()
            macc = psbm("macc", [128, 32]); t_macc = tk()
            A("dve", P(nc.vector.memset, macc[:], 0.0), w=[t_macc])
            with ExitStack() as ph:
                sch.barrier()
                def psb(name, shape, dt=F32):
                    return ph.enter_context(nc.sbuf_tensor(uname(name), shape, dt))
                wo = psb("wo", [128, KC, D], BF16); t_wo = tk()
                for c0 in range(0, KC, 4):
                    A("pool", (P(nc.gpsimd.dma_start, out=wo[:, c0:c0 + 4, :], in_=wout_d[l, c0 * 128:(c0 + 4) * 128, :].rearrange("(c p) n -> p c n", p=128))), w=[t_wo], dma=True, key="wo")
                gbcs = {}
                ycR = Rot(psb, tk, "yc", [128, 512], F32, 4)
                ysqR = Rot(psb, tk, "ysq", [128, 512], BF16, 3)
                rgR = Rot(psb, tk, "rg", [128, 3, 512], F32, 2)
                ynbR = Rot(psb, tk, "ynb", [128, KC, 512], BF16, 2)
                xrR = Rot(psb, tk, "xr", [128, D], F32, 2)
                h2R = Rot(psb, tk, "h2d", [128, 512], F32, 3)
                groups3 = [(0, 8), (8, 12), (12, 16)]

                def ld_chunk(b, c, t0, n):
                    yc, t_yc = ycR.next(); key = "yc%d" % (ycR.i % 4)
                    A("sp", (P(nc.sync.dma_start, out=yc[:, 0:n], in_=YT[b, c, :, t0:t0 + n])), r=[t_YT[b][c]], w=[t_yc], dma=True, key=key)
                    return yc, t_yc

                def d1_item(b, t0, n, gbc, t_gbc):
                    rg, t_rg = rgR.next(); ynb, t_ynb = ynbR.next()
                    for gi, (c0, c1) in enumerate(groups3):
                        pt, ptk = psnext()
                        for c in range(c0, c1):
                            yc, t_yc = ld_chunk(b, c, t0, n)
                            ysq, t_ysq = ysqR.next()
                            A("act", (P(nc.scalar.activation, out=ysq[:, 0:n], in_=yc[:, 0:n], func=AF.Square)), r=[t_yc], w=[t_ysq])
                            A("pe", (P(nc.tensor.matmul, pt[:, 0:n], lhsT=onesb[:], rhs=ysq[:, 0:n], start=(c == c0), stop=(c == c1 - 1))), r=[t_ysq, t_onesb], w=[ptk])
                        A("act", (P(nc.scalar.activation, out=rg[:, gi, 0:n], in_=pt[:, 0:n], func=AF.Sqrt, scale=1.0 / (128 * (c1 - c0)), bias=eps_t[:, 0:1])), r=[ptk, t_eps], w=[t_rg])
                        A("dve", (P(nc.vector.reciprocal, out=rg[:, gi, 0:n], in_=rg[:, gi, 0:n])), r=[t_rg], w=[t_rg])
                    for gi, (c0, c1) in enumerate(groups3):
                        for c in range(c0, c1):
                            yc, t_yc = ld_chunk(b, c, t0, n)
                            A("dve", (P(nc.vector.scalar_tensor_tensor, out=ynb[:, c, 0:n], in0=yc[:, 0:n], scalar=pv[l][:, PV_GG + c:PV_GG + c + 1], in1=rg[:, gi, 0:n], op0=ALU.mult, op1=ALU.mult)), r=[t_yc, t_rg, t_pv[l]], w=[t_ynb])
                    yield
                    for tj in range(n // 128):
                        tt = t0 // 128 + tj
                        xr, t_xr = xrR.next(); xkey = "xr%d" % (xrR.i % 2)
                        A("sp", (P(nc.sync.dma_start, out=xr[:], in_=resid_src(l, b, tt))), r=[state["rtok"][b][tt]], w=[t_xr], dma=True, key=xkey)
                        for db in range(4):
                            pt, ptk = psnext()
                            for c in range(KC):
                                A("pe", (P(nc.tensor.matmul, pt[:, :], lhsT=ynb[:, c, tj * 128:(tj + 1) * 128], rhs=wo[:, c, db * 512:(db + 1) * 512], start=(c == 0), stop=(c == KC - 1))), r=[t_ynb, t_wo], w=[ptk])
                            h2, t_h2 = h2R.next()
                            A("dve", (P(nc.vector.tensor_tensor, out=h2[:, :], in0=pt[:, :], in1=gbc[:, db * 512:(db + 1) * 512], op=ALU.mult)), r=[ptk, t_gbc], w=[t_h2])
                            A("pool", (P(nc.gpsimd.tensor_tensor, out=xr[:, db * 512:(db + 1) * 512], in0=xr[:, db * 512:(db + 1) * 512], in1=h2[:, :], op=ALU.add)), r=[t_h2, t_xr], w=[t_xr])
                        nt_ = tk("R"); state["rtok"][b][tt] = nt_
                        A("sp", (P(nc.sync.dma_start, out=R[b, tt * 128:(tt + 1) * 128, :], in_=xr[:])), r=[t_xr], w=[nt_], dma=True, key=xkey + "s")

                def d1_items():
                    for b in range(NB):
                        for (t0, n) in tgs:
                            if last and t0 >= S:
                                continue
                            r3 = b if t0 < S else 2
                            gk = "g%d" % r3
                            if gk not in gbcs:
                                gbcs[gk] = [psb("gbc" + gk, [128, D]), tk(), None]
                            if gbcs[gk][2] != r3:
                                gbcs[gk][2] = r3
                                A("sp", (P(nc.sync.dma_start, out=gbcs[gk][0][:], in_=MOD[l, r3, 2 * D:3 * D].partition_broadcast(128))), r=[t_MODs[l]], w=[gbcs[gk][1]], dma=True, key="gbc" + gk)
                            gbc, t_gbc = gbcs[gk][0], gbcs[gk][1]
                            yield d1_item(b, t0, n, gbc, t_gbc)
                pipeline(d1_items())
            with ExitStack() as ph:
                sch.barrier()
                def psb(name, shape, dt=F32):
                    return ph.enter_context(nc.sbuf_tensor(uname(name), shape, dt))
                wrt = psb("wrt", [128, KC, 36]); t_wrt = tk()
                A("sp", P(nc.sync.dma_start, out=wrt[:], in_=wr_d[l].rearrange("(c p) n -> p c n", p=128)), w=[t_wrt], dma=True, key="wrt")
                rbt = psb("rbt", [128, 36]); t_rbt = tk()
                A("sp", P(nc.sync.dma_start, out=rbt[:], in_=rb_d[l, 0].partition_broadcast(128)), w=[t_rbt], dma=True, key="rbt")
                xrR = Rot(psb, tk, "xr2", [128, D], F32, 3)
                h2R = Rot(psb, tk, "h2", [128, D], F32, 3)
                h2TR = Rot(psb, tk, "h2T", [128, KC, 128], F32, 3)
                h2bR = Rot(psb, tk, "h2b", [128, D], BF16, 2)
                junkR = Rot(psb, tk, "junkd", [128, D], BF16, 2)
                stR = Rot(psb, tk, "std", [128, 4], F32, 4)
                lgR = Rot(psb, tk, "lg", [128, 36], F32, 3)
                rtR = Rot(psb, tk, "rt", [128, 64], F32, 4)
                selR = Rot(psb, tk, "sel", [128, 8], F32, 2)
                top8R = Rot(psb, tk, "top8", [128, 8], F32, 2)
                ogR = Rot(psb, tk, "og", [128, 4], F32, 4)
                ohR = Rot(psb, tk, "oh", [128, 2, 8], F32, 4)
                mbR = Rot(psb, tk, "mb", [128, 32], BF16, 4)
                maccbR = Rot(psb, tk, "maccb", [128, 32], BF16, 3)
                cumR = Rot(psb, tk, "cum", [128, 32], F32, 3)

                def d2_item(b, tt):
                    gti = b * NTM + tt
                    r3 = b if tt < S // 128 else 2
                    xr, t_xr = xrR.next(); xkey = "xr2_%d" % (xrR.i % 3)
                    junk, t_junk = junkR.next(); st, t_st = stR.next()
                    A("sp", (P(nc.sync.dma_start, out=xr[:], in_=R[b, tt * 128:(tt + 1) * 128, :])), r=[state["rtok"][b][tt]], w=[t_xr], dma=True, key=xkey)
                    A("act", (P(nc.scalar.activation, out=junk[:], in_=xr[:], func=AF.Square, accum_out=st[:, 0:1])), r=[t_xr], w=[t_junk, t_st])
                    A("act", (P(nc.scalar.activation, out=st[:, 1:2], in_=st[:, 0:1], func=AF.Sqrt, scale=1.0 / D, bias=eps_t[:, 0:1])), r=[t_st, t_eps], w=[t_st])
                    A("dve", (P(nc.vector.reciprocal, out=st[:, 2:3], in_=st[:, 1:2])), r=[t_st], w=[t_st])
                    yield
                    h2, t_h2 = h2R.next()
                    A("act", (P(nc.scalar.activation, out=h2[:], in_=xr[:], func=AF.Identity, scale=st[:, 2:3])), r=[t_xr, t_st], w=[t_h2])
                    yield
                    pts = []
                    for c0 in range(0, KC, 4):
                        pt, ptk = psnext(); pts.append((pt, ptk))
                        for c in range(c0, c0 + 4):
                            A("pe", (P(nc.tensor.transpose, pt[:, (c - c0) * 128:(c - c0 + 1) * 128], h2[:, c * 128:(c + 1) * 128], ident[:])), r=[t_h2, t_ident], w=[ptk])
                    h2T, t_h2T = h2TR.next()
                    for c0 in range(0, KC, 4):
                        pt, ptk = pts[c0 // 4]
                        for c in range(c0, c0 + 4):
                            sc_ap = Avec[l][1][:, r3 * KC + c:r3 * KC + c + 1]
                            bi_ap = modT[l][:, (3 * 16 + c) * 3 + r3:(3 * 16 + c) * 3 + r3 + 1]
                            if c < 8:
                                A("dve", (P(nc.vector.tensor_scalar, out=h2T[:, c, :], in0=pt[:, (c - c0) * 128:(c - c0 + 1) * 128], scalar1=sc_ap, scalar2=bi_ap, op0=ALU.mult, op1=ALU.add)), r=[ptk, t_Av[l], t_modT[l]], w=[t_h2T])
                            else:
                                A("act", (P(nc.scalar.activation, out=h2T[:, c, :], in_=pt[:, (c - c0) * 128:(c - c0 + 1) * 128], func=AF.Identity, scale=sc_ap, bias=bi_ap)), r=[ptk, t_Av[l], t_modT[l]], w=[t_h2T])
                    yield
                    pl, plk = psnext()
                    for c in range(KC):
                        A("pe", (P(nc.tensor.matmul, pl[:, 0:36], lhsT=h2T[:, c, :], rhs=wrt[:, c, :], start=(c == 0), stop=(c == KC - 1))), r=[t_h2T, t_wrt], w=[plk])
                    pts = []
                    for c0 in range(0, KC, 4):
                        pt, ptk = psnext(); pts.append((pt, ptk))
                        for c in range(c0, c0 + 4):
                            A("pe", (P(nc.tensor.transpose, pt[:, (c - c0) * 128:(c - c0 + 1) * 128], h2T[:, c, :], ident[:])), r=[t_h2T, t_ident], w=[ptk])
                    lg, t_lg = lgR.next(); rt, t_rt = rtR.next(); sel, t_sel = selR.next(); top8, t_top8 = top8R.next()
                    og, t_og = ogR.next(); oh, t_oh = ohR.next(); mb, t_mb = mbR.next()
                    A("dve", (P(nc.vector.tensor_tensor, out=lg[:], in0=pl[:, 0:36], in1=rbt[:], op=ALU.add)), r=[plk, t_rbt], w=[t_lg])
                    h2b, t_h2b = h2bR.next(); hkey = "h2b%d" % (h2bR.i % 2)
                    for c0 in range(0, KC, 4):
                        pt, ptk = pts[c0 // 4]
                        A("act", (P(nc.scalar.activation, func=AF.Copy, out=h2b[:, c0 * 128:(c0 + 4) * 128], in_=pt[:, :])), r=[ptk], w=[t_h2b])
                    A("sp", (P(nc.sync.dma_start, out=H2[gti * 128:(gti + 1) * 128, :], in_=h2b[:])), r=[t_h2b], w=[t_H2], dma=True, key=hkey)
                    yield
                    A("dve", (P(nc.vector.tensor_reduce, out=rt[:, 0:1], in_=lg[:, 0:4], axis=AX.X, op=ALU.max)), r=[t_lg], w=[t_rt])
                    A("dve", (P(nc.vector.tensor_scalar, out=og[:], in0=lg[:, 0:4], scalar1=rt[:, 0:1], scalar2=None, op0=ALU.is_equal)), r=[t_lg, t_rt], w=[t_og])
                    A("dve", (P(nc.vector.tensor_scalar, out=rt[:, 1:2], in0=rt[:, 0:1], scalar1=-1.0, scalar2=None, op0=ALU.mult)), r=[t_rt], w=[t_rt])
                    A("act", (P(nc.scalar.activation, out=rt[:, 4:8], in_=lg[:, 0:4], func=AF.Exp, bias=rt[:, 1:2], accum_out=rt[:, 2:3])), r=[t_lg, t_rt], w=[t_rt])
                    A("dve", (P(nc.vector.tensor_scalar, out=sel[:], in0=lg[:, 4:12], scalar1=og[:, 0:1], scalar2=None, op0=ALU.mult)), r=[t_lg, t_og], w=[t_sel])
                    for g4 in range(1, 4):
                        A("dve", (P(nc.vector.scalar_tensor_tensor, out=sel[:], in0=lg[:, 4 + 8 * g4:12 + 8 * g4], scalar=og[:, g4:g4 + 1], in1=sel[:], op0=ALU.mult, op1=ALU.add)), r=[t_lg, t_og, t_sel], w=[t_sel])
                    A("dve", (P(nc.vector.max, out=top8[:], in_=sel[:])), r=[t_sel], w=[t_top8])
                    A("dve", (P(nc.vector.tensor_scalar, out=oh[:, 0, :], in0=sel[:], scalar1=top8[:, 0:1], scalar2=None, op0=ALU.is_equal)), r=[t_sel, t_top8], w=[t_oh])
                    A("dve", (P(nc.vector.tensor_scalar, out=oh[:, 1, :], in0=sel[:], scalar1=top8[:, 1:2], scalar2=None, op0=ALU.is_equal)), r=[t_sel, t_top8], w=[t_oh])
                    A("dve", (P(nc.vector.tensor_tensor, out=rt[:, 8:9], in0=top8[:, 1:2], in1=top8[:, 0:1], op=ALU.subtract)), r=[t_top8, t_rt], w=[t_rt])
                    A("act", (P(nc.scalar.activation, out=rt[:, 9:10], in_=rt[:, 8:9], func=AF.Exp)), r=[t_rt], w=[t_rt])
                    for g4 in range(4):
                        A("dve", (P(nc.vector.tensor_scalar, out=M1[:, gti, g4 * 8:(g4 + 1) * 8], in0=oh[:, 0, :], scalar1=og[:, g4:g4 + 1], scalar2=None, op0=ALU.mult)), r=[t_oh, t_og], w=[t_M])
                        A("dve", (P(nc.vector.tensor_scalar, out=M2[:, gti, g4 * 8:(g4 + 1) * 8], in0=oh[:, 1, :], scalar1=og[:, g4:g4 + 1], scalar2=None, op0=ALU.mult)), r=[t_oh, t_og], w=[t_M])
                    A("dve", (P(nc.vector.tensor_tensor, out=mb[:], in0=M1[:, gti, :], in1=M2[:, gti, :], op=ALU.add)), r=[t_M], w=[t_mb])
                    yield
                    A("dve", (P(nc.vector.reciprocal, out=rt[:, 3:4], in_=rt[:, 2:3])), r=[t_rt], w=[t_rt])
                    A("dve", (P(nc.vector.tensor_scalar, out=rt[:, 10:11], in0=rt[:, 9:10], scalar1=1.0, scalar2=None, op0=ALU.add)), r=[t_rt], w=[t_rt])
                    A("dve", (P(nc.vector.reciprocal, out=rt[:, 11:12], in_=rt[:, 10:11])), r=[t_rt], w=[t_rt])
                    A("dve", (P(nc.vector.tensor_tensor, out=RW[:, gti, 0:1], in0=rt[:, 11:12], in1=rt[:, 3:4], op=ALU.mult)), r=[t_rt], w=[t_RW])
                    A("dve", (P(nc.vector.tensor_tensor, out=RW[:, gti, 1:2], in0=rt[:, 3:4], in1=RW[:, gti, 0:1], op=ALU.subtract)), r=[t_rt, t_RW], w=[t_RW])
                    maccb, t_maccb = maccbR.next(); cum, t_cum = cumR.next()
                    pc_, pck_ = psnext()
                    A("dve", (P(nc.vector.tensor_copy, out=maccb[:], in_=macc[:])), r=[t_macc], w=[t_maccb])
                    A("dve", (P(nc.vector.tensor_tensor, out=macc[:], in0=macc[:], in1=mb[:], op=ALU.add)), r=[t_macc, t_mb], w=[t_macc])
                    A("pe", (P(nc.tensor.matmul, pc_[:, 0:32], lhsT=ustr[:], rhs=mb[:], start=True, stop=False)), r=[t_ustr, t_mb], w=[pck_])
                    A("pe", (P(nc.tensor.matmul, pc_[:, 0:32], lhsT=onesb[:], rhs=maccb[:], start=False, stop=True)), r=[t_onesb, t_maccb], w=[pck_])
                    yield
                    A("dve", (P(nc.vector.tensor_copy, out=cum[:], in_=pc_[:, 0:32])), r=[pck_], w=[t_cum])
                    A("dve", (P(nc.vector.tensor_tensor, out=rt[:, 16:48], in0=cum[:], in1=M1[:, gti, :], op=ALU.mult)), r=[t_cum, t_M, t_rt], w=[t_rt])
                    A("dve", (P(nc.vector.tensor_reduce, out=RK[:, gti, 0:1], in_=rt[:, 16:48], axis=AX.X, op=ALU.add)), r=[t_rt], w=[t_RK])
                    A("dve", (P(nc.vector.tensor_tensor, out=rt[:, 16:48], in0=cum[:], in1=M2[:, gti, :], op=ALU.mult)), r=[t_cum, t_M, t_rt], w=[t_rt])
                    A("dve", (P(nc.vector.tensor_reduce, out=RK[:, gti, 1:2], in_=rt[:, 16:48], axis=AX.X, op=ALU.add)), r=[t_rt], w=[t_RK])
                pipeline(d2_item(b, tt) for b in range(NB) for tt in range(NTM))
            if stop_after == "D" and l == 0:
                break
            NGT = NB * NTM
            cnt = psbm("cnt", [128, 32]); t_cnt = tk()
            pad = psbm("pad", [128, 32]); pstart = psbm("pstart", [128, 33]); t_pad = tk()
            padi = psbm("padi", [128, 32], I32); t_padi = tk()
            DSTf = psbm("DSTf", [128, NGT, 2]); DST = psbm("DST", [128, NGT, 2], U32); t_DST = tk()
            ebf = psbm("ebf", [128, NBLK]); t_ebf = tk()
            widx = psbm("widx", [128, NBLK, 3], U32); widxf = psbm("widxf", [128, NBLK, 3]); t_widx = tk()
            scr = psbm("scr", [128, 32]); t_scr = tk()
            pc_, pck_ = psnext()
            A("pe", (P(nc.tensor.matmul, pc_[:, 0:32], lhsT=onesf[:], rhs=macc[:], start=True, stop=True)), r=[t_onesf, t_macc], w=[pck_])
            A("dve", (P(nc.vector.tensor_scalar, out=cnt[:], in0=pc_[:, 0:32], scalar1=float(BLK - 1), scalar2=1.0 / BLK, op0=ALU.add, op1=ALU.mult)), r=[pck_], w=[t_cnt])
            A("dve", (P(nc.vector.tensor_copy, out=padi[:], in_=cnt[:])), r=[t_cnt], w=[t_padi])
            A("dve", (P(nc.vector.tensor_copy, out=pad[:], in_=padi[:])), r=[t_padi], w=[t_pad])
            A("dve", (P(nc.vector.tensor_tensor, out=scr[:], in0=pad[:], in1=cnt[:], op=ALU.is_gt)), r=[t_pad, t_cnt], w=[t_scr])
            A("dve", (P(nc.vector.tensor_tensor, out=pad[:], in0=pad[:], in1=scr[:], op=ALU.subtract)), r=[t_pad, t_scr], w=[t_pad])
            A("dve", (P(nc.vector.tensor_scalar, out=pad[:], in0=pad[:], scalar1=float(BLK), scalar2=None, op0=ALU.mult)), r=[t_pad], w=[t_pad])
            A("dve", (P(nc.vector.memset, pstart[:, 0:1], 0.0)), w=[t_pad])
            for e in range(32):
                A("dve", (P(nc.vector.tensor_tensor, out=pstart[:, e + 1:e + 2], in0=pstart[:, e:e + 1], in1=pad[:, e:e + 1], op=ALU.add)), r=[t_pad], w=[t_pad])
            for gti in range(NGT):
                for k, Mk in enumerate((M1, M2)):
                    A("dve", (P(nc.vector.tensor_tensor, out=scr[:], in0=pstart[:, 0:32], in1=Mk[:, gti, :], op=ALU.mult)), r=[t_pad, t_M], w=[t_scr])
                    A("dve", (P(nc.vector.tensor_reduce, out=DSTf[:, gti, k:k + 1], in_=scr[:], axis=AX.X, op=ALU.add)), r=[t_scr], w=[t_DST])
            A("dve", (P(nc.vector.tensor_tensor, out=DSTf[:], in0=DSTf[:], in1=RK[:], op=ALU.add)), r=[t_DST, t_RK], w=[t_DST])
            A("dve", (P(nc.vector.tensor_copy, out=DST[:], in_=DSTf[:])), r=[t_DST], w=[t_DST])
            for bk in range(NBLK):
                A("dve", (P(nc.vector.tensor_scalar, out=scr[:], in0=pstart[:, 1:33], scalar1=float(bk * BLK), scalar2=None, op0=ALU.is_le)), r=[t_pad], w=[t_scr])
                A("dve", (P(nc.vector.tensor_reduce, out=ebf[:, bk:bk + 1], in_=scr[:], axis=AX.X, op=ALU.add)), r=[t_scr], w=[t_ebf])
            A("dve", (P(nc.vector.tensor_scalar, out=ebf[:], in0=ebf[:], scalar1=31.0, scalar2=None, op0=ALU.min)), r=[t_ebf], w=[t_ebf])
            A("dve", (P(nc.vector.tensor_scalar, out=widxf[:, :, 0], in0=ebf[:], scalar1=256.0, scalar2=pidx[:, 0:1], op0=ALU.mult, op1=ALU.add)), r=[t_ebf, t_pidx], w=[t_widx])
            A("dve", (P(nc.vector.tensor_scalar, out=widxf[:, :, 1], in0=widxf[:, :, 0], scalar1=128.0, scalar2=None, op0=ALU.add)), r=[t_widx], w=[t_widx])
            A("dve", (P(nc.vector.tensor_scalar, out=widxf[:, :, 2], in0=ebf[:], scalar1=128.0, scalar2=pidx[:, 0:1], op0=ALU.mult, op1=ALU.add)), r=[t_ebf, t_pidx], w=[t_widx])
            A("dve", (P(nc.vector.tensor_scalar, out=widxf[:, :, 0:2], in0=widxf[:, :, 0:2], scalar1=float(l * NE * 256), scalar2=None, op0=ALU.add)), r=[t_widx], w=[t_widx])
            A("dve", (P(nc.vector.tensor_scalar, out=widxf[:, :, 2], in0=widxf[:, :, 2], scalar1=float(l * NE * 128), scalar2=None, op0=ALU.add)), r=[t_widx], w=[t_widx])
            A("dve", (P(nc.vector.tensor_copy, out=widx[:], in_=widxf[:])), r=[t_widx], w=[t_widx])
            t_XSs = []
            with ExitStack() as ph:
                sch.barrier()
                def psb(name, shape, dt=F32):
                    return ph.enter_context(nc.sbuf_tensor(uname(name), shape, dt))
                hbR = Rot(psb, tk, "hb", [128, D], BF16, 2)
                def e_item(gti):
                    hb, t_hb = hbR.next(); hk = "hb%d" % (hbR.i % 2)
                    A("sp", (P(nc.sync.dma_start, out=hb[:], in_=H2[gti * 128:(gti + 1) * 128, :])), r=[t_H2], w=[t_hb], dma=True, key=hk)
                    yield
                    for k in range(2):
                        A("pool", (P(nc.gpsimd.indirect_dma_start, out=XS, out_offset=bass.IndirectOffsetOnAxis(ap=DST[:, gti, k:k + 1], axis=0), in_=hb[:], in_offset=None)), r=[t_hb, t_DST], w=[t_XSs.append(tk("XS")) or t_XSs[-1]], dma=True, key=hk + "s%d" % k)
                pipeline(e_item(gti) for gti in range(NGT))
            t_YSs = []
            with ExitStack() as ph:
                sch.barrier()
                def psb(name, shape, dt=F32):
                    return ph.enter_context(nc.sbuf_tensor(uname(name), shape, dt))
                xbR = Rot(psb, tk, "xb", [128, 2, D], BF16, 2)
                xTR = Rot(psb, tk, "xTe", [128, KC, BLK], BF16, 2)
                wgR = Rot(psb, tk, "wgt", [128, KC * 256], BF16, 4)
                wuR = Rot(psb, tk, "wut", [128, KC * 256], BF16, 4)
                wdR = Rot(psb, tk, "wdt", [128, 4 * D], BF16, 2)
                sgR = Rot(psb, tk, "sg", [128, BLK], F32, 3)
                aTR = Rot(psb, tk, "aT", [128, 4, BLK], BF16, 2)
                ybR = Rot(psb, tk, "yb", [128, D], F32, 2)
                wgf = wg_d.rearrange("l r c -> (l r) c"); wuf = wu_d.rearrange("l r c -> (l r) c"); wdf = wd_d.rearrange("l r c -> (l r) c")

                def f_item(bk):
                    xb, t_xb = xbR.next(); xkey = "xb%d" % (xbR.i % 2)
                    A("sp", (P(nc.sync.dma_start, out=xb[:], in_=XS[bk * BLK:(bk + 1) * BLK, :].rearrange("(j p) d -> p j d", p=128))), r=t_XSs, w=[t_xb], dma=True, key=xkey)
                    yield
                    wd_, t_wd_ = wdR.next(); dkey = "wdt%d" % (wdR.i % 2)
                    A("pool", (P(nc.gpsimd.indirect_dma_start, out=wd_[:], out_offset=None, in_=wdf, in_offset=bass.IndirectOffsetOnAxis(ap=widx[:, bk, 2:3], axis=0))), r=[t_widx], w=[t_wd_], dma=True, key=dkey)
                    halves = []
                    for fh in range(2):
                        wg_, t_wg_ = wgR.next(); gkey = "wgt%d" % (wgR.i % 4)
                        wu_, t_wu_ = wuR.next(); ukey = "wut%d" % (wuR.i % 4)
                        A("pool", (P(nc.gpsimd.indirect_dma_start, out=wg_[:], out_offset=None, in_=wgf, in_offset=bass.IndirectOffsetOnAxis(ap=widx[:, bk, fh:fh + 1], axis=0))), r=[t_widx], w=[t_wg_], dma=True, key=gkey)
                        A("pool", (P(nc.gpsimd.indirect_dma_start, out=wu_[:], out_offset=None, in_=wuf, in_offset=bass.IndirectOffsetOnAxis(ap=widx[:, bk, fh:fh + 1], axis=0))), r=[t_widx], w=[t_wu_], dma=True, key=ukey)
                        halves.append((wg_, t_wg_, wu_, t_wu_))
                    xT, t_xT = xTR.next()
                    for j in range(2):
                        for c0 in range(0, KC, 8):
                            pt, ptk = psnext()
                            ptb = pt[:].bitcast(BF16)
                            for c in range(c0, c0 + 8):
                                A("pe", (P(nc.tensor.transpose, ptb[:, (c - c0) * 128:(c - c0 + 1) * 128], xb[:, j, c * 128:(c + 1) * 128], identb[:])), r=[t_xb, t_identb], w=[ptk])
                            if (j + c0 // 8) % 2 == 0:
                                A("act", (P(nc.scalar.activation, func=AF.Copy, out=xT[:, c0:c0 + 8, j * 128:(j + 1) * 128], in_=ptb[:, 0:1024].rearrange("p (c t) -> p c t", t=128))), r=[ptk], w=[t_xT])
                            else:
                                A("dve", (P(nc.vector.tensor_copy, out=xT[:, c0:c0 + 8, j * 128:(j + 1) * 128], in_=ptb[:, 0:1024].rearrange("p (c t) -> p c t", t=128))), r=[ptk], w=[t_xT])
                    yield
                    aT, t_aT = aTR.next()
                    for fh in range(2):
                        wg_, t_wg_, wu_, t_wu_ = halves[fh]
                        for fl in range(2):
                            fc = fh * 2 + fl
                            pg, pgk = psnext(); pu, puk = psnext()
                            for c in range(KC):
                                A("pe", (P(nc.tensor.matmul, pg[:, 0:BLK], lhsT=wg_[:, c * 256 + fl * 128: c * 256 + (fl + 1) * 128], rhs=xT[:, c, :], start=(c == 0), stop=(c == KC - 1))), r=[t_wg_, t_xT], w=[pgk])
                            for c in range(KC):
                                A("pe", (P(nc.tensor.matmul, pu[:, 0:BLK], lhsT=wu_[:, c * 256 + fl * 128: c * 256 + (fl + 1) * 128], rhs=xT[:, c, :], start=(c == 0), stop=(c == KC - 1))), r=[t_wu_, t_xT], w=[puk])
                            sg, t_sg = sgR.next()
                            A("act", (P(nc.scalar.activation, out=sg[:], in_=pg[:, 0:BLK], func=AF.Silu)), r=[pgk], w=[t_sg])
                            A("dve", (P(nc.vector.tensor_tensor, out=aT[:, fc, :], in0=sg[:], in1=pu[:, 0:BLK], op=ALU.mult)), r=[t_sg, puk], w=[t_aT])
                    for j in range(2):
                        yb, t_yb = ybR.next(); ykey = "yb%d" % (ybR.i % 2)
                        for db in range(4):
                            pt, ptk = psnext()
                            for fc in range(4):
                                A("pe", (P(nc.tensor.matmul, pt[:, :], lhsT=aT[:, fc, j * 128:(j + 1) * 128], rhs=wd_[:, fc * D + db * 512: fc * D + (db + 1) * 512], start=(fc == 0), stop=(fc == 3))), r=[t_aT, t_wd_], w=[ptk])
                            if db % 2 == 0:
                                A("act", (P(nc.scalar.activation, func=AF.Copy, out=yb[:, db * 512:(db + 1) * 512], in_=pt[:, :])), r=[ptk], w=[t_yb])
                            else:
                                A("dve", (P(nc.vector.tensor_copy, out=yb[:, db * 512:(db + 1) * 512], in_=pt[:, :])), r=[ptk], w=[t_yb])
                        A("sp", (P(nc.sync.dma_start, out=YS[bk * BLK + j * 128: bk * BLK + (j + 1) * 128, :], in_=yb[:])), r=[t_yb], w=[t_YSs.append(tk("YS")) or t_YSs[-1]], dma=True, key=ykey)
                pipeline(f_item(bk) for bk in range(NBLK))
            with ExitStack() as ph:
                sch.barrier()
                def psb(name, shape, dt=F32):
                    return ph.enter_context(nc.sbuf_tensor(uname(name), shape, dt))
                y0R = Rot(psb, tk, "y0", [128, D], F32, 3)
                y1R = Rot(psb, tk, "y1", [128, D], F32, 3)
                xgR = Rot(psb, tk, "xg", [128, D], F32, 4)
                junkR = Rot(psb, tk, "junkg", [128, D], BF16, 2)
                stR = Rot(psb, tk, "stg", [128, 4], F32, 3)
                g2s = {}
                if last:
                    fgb = psb("fgb", [128, D]); t_fgb = tk()
                    A("sp", P(nc.sync.dma_start, out=fgb[:], in_=fg_d[0].partition_broadcast(128)), w=[t_fgb], dma=True, key="fgb")

                def g_item(b, tt, g2, t_g2):
                    gti = b * NTM + tt
                    y0, t_y0 = y0R.next(); k0_ = "y0_%d" % (y0R.i % 3)
                    y1, t_y1 = y1R.next(); k1_ = "y1_%d" % (y1R.i % 3)
                    xr, t_xr = xgR.next(); kx_ = "xg%d" % (xgR.i % 4)
                    A("pool", (P(nc.gpsimd.indirect_dma_start, out=y0[:], out_offset=None, in_=YS, in_offset=bass.IndirectOffsetOnAxis(ap=DST[:, gti, 0:1], axis=0))), r=t_YSs + [t_DST], w=[t_y0], dma=True, key=k0_)
                    A("pool", (P(nc.gpsimd.indirect_dma_start, out=y1[:], out_offset=None, in_=YS, in_offset=bass.IndirectOffsetOnAxis(ap=DST[:, gti, 1:2], axis=0))), r=t_YSs + [t_DST], w=[t_y1], dma=True, key=k1_)
                    A("sp", (P(nc.sync.dma_start, out=xr[:], in_=R[b, tt * 128:(tt + 1) * 128, :])), r=[state["rtok"][b][tt]], w=[t_xr], dma=True, key=kx_)
                    yield
                    yield
                    A("act", (P(nc.scalar.activation, out=y0[:], in_=y0[:], func=AF.Identity, scale=RW[:, gti, 0:1])), r=[t_y0, t_RW], w=[t_y0])
                    A("dve", (P(nc.vector.scalar_tensor_tensor, out=y0[:], in0=y1[:], scalar=RW[:, gti, 1:2], in1=y0[:], op0=ALU.mult, op1=ALU.add)), r=[t_y0, t_y1, t_RW], w=[t_y0])
                    A("pool", (P(nc.gpsimd.tensor_tensor, out=y0[:], in0=y0[:], in1=g2[:], op=ALU.mult)), r=[t_y0, t_g2], w=[t_y0])
                    A("dve", (P(nc.vector.tensor_tensor, out=xr[:], in0=xr[:], in1=y0[:], op=ALU.add)), r=[t_y0, t_xr], w=[t_xr])
                    if last:
                        junk, t_junk = junkR.next(); st, t_st = stR.next()
                        A("act", (P(nc.scalar.activation, out=junk[:], in_=xr[:], func=AF.Square, accum_out=st[:, 0:1])), r=[t_xr], w=[t_junk, t_st])
                        A("act", (P(nc.scalar.activation, out=st[:, 1:2], in_=st[:, 0:1], func=AF.Sqrt, scale=1.0 / D, bias=eps_t[:, 0:1])), r=[t_st, t_eps], w=[t_st])
                        A("dve", (P(nc.vector.reciprocal, out=st[:, 2:3], in_=st[:, 1:2])), r=[t_st], w=[t_st])
                        A("dve", (P(nc.vector.scalar_tensor_tensor, out=xr[:], in0=xr[:], scalar=st[:, 2:3], in1=fgb[:], op0=ALU.mult, op1=ALU.mult)), r=[t_xr, t_st, t_fgb], w=[t_xr])
                    yield
                    if not last:
                        nt_ = tk("R"); state["rtok"][b][tt] = nt_
                        A("sp", (P(nc.sync.dma_start, out=R[b, tt * 128:(tt + 1) * 128, :], in_=xr[:])), r=[t_xr], w=[nt_], dma=True, key=kx_ + "s")
                    else:
                        nt_ = tk("O")
                        A("sp", (P(nc.sync.dma_start, out=out_d[b, tt * 128:(tt + 1) * 128, :], in_=xr[:])), r=[t_xr], w=[nt_], dma=True, key=kx_ + "s")

                def g_items():
                    for b in range(NB):
                        for tt in range(NTM):
                            r3 = b if tt < S // 128 else 2
                            if r3 not in g2s:
                                g2 = psb("g2_%d" % r3, [128, D]); t_g2 = tk()
                                A("sp", (P(nc.sync.dma_start, out=g2[:], in_=MOD[l, r3, 5 * D:6 * D].partition_broadcast(128))), r=[t_MODs[l]], w=[t_g2], dma=True, key="g2_%d" % r3)
                                g2s[r3] = (g2, t_g2)
                            yield g_item(b, tt, *g2s[r3])
                pipeline(g_items())
        if stop_after is not None and l == 0:
            break

    if dbg:
        with ExitStack() as ph:
            sch.barrier()
            dt_ = ph.enter_context(nc.sbuf_tensor(uname("dbgt"), [128, D], F32)); t_dt = tk()
            for b in range(NB):
                for tt in range(NTT):
                    A("sp", (P(nc.sync.dma_start, out=dt_[:], in_=R[b, tt * 128:(tt + 1) * 128, :])), r=[state["rtok"][b][tt]], w=[t_dt], dma=True, key="dbgl")
                    A("sp", (P(nc.sync.dma_start, out=dbg_d[b, tt * 128:(tt + 1) * 128, :], in_=dt_[:])), r=[t_dt], w=[tk()], dma=True, key="dbgs")
    nsem = sch.emit()
    es.close()
    return nc


def _fm(v):
    return np.ascontiguousarray(v.reshape(-1, 128).T)


def prep_shared(inp, S):
    sh = {}
    sh["w_ada"] = np.ascontiguousarray(inp["w_ada"]); sh["b_ada"] = np.ascontiguousarray(inp["b_ada"][:, None, :])
    pvs = []
    for l in range(2):
        pv = np.zeros((128, 256), np.float32)
        pv[:, 0:16] = _fm(inp["norm1_g"][l]); pv[:, 16:32] = _fm(inp["norm2_g"][l]); pv[:, 32:48] = _fm(inp["grp_norm_g"][l])
        pv[:, 48] = inp["q_norm_g"][l]; pv[:, 49] = inp["k_norm_g"][l]
        for k in range(3):
            pv[:, 50 + k * 4: 50 + k * 4 + 4] = _fm(inp["sconv_w"][l, k])
        for k in range(31):
            pv[:, 62 + k * 4: 62 + k * 4 + 4] = _fm(inp["conf_dw_w"][l, k])
        pv[:, 186:190] = _fm(inp["conf_dw_b"][l]); pv[:, 190:194] = _fm(inp["conf_ln_g"][l]); pv[:, 194:198] = _fm(inp["conf_ln_b"][l])
        pvs.append(pv)
    sh["pv"] = np.stack(pvs)
    sh["fg"] = np.ascontiguousarray(inp["final_g"][None, :])
    cols = list(range(0, 1024)) + list(range(1024, 1536))
    for j in range(4):
        cols += list(range(1536 + j * 128, 1536 + (j + 1) * 128))
        cols += list(range(2048 + j * 128, 2048 + (j + 1) * 128))
        cols += list(range(2560 + j * 128, 2560 + (j + 1) * 128))
    for j in range(4):
        cols += list(range(3072 + j * 128, 3072 + (j + 1) * 128))
        cols += list(range(3584 + j * 128, 3584 + (j + 1) * 128))
    sh["w_in"] = np.ascontiguousarray(inp["w_in"][:, :, cols])
    sh["w_out"] = np.ascontiguousarray(inp["w_out"])
    sh["wr"] = np.ascontiguousarray(np.concatenate([inp["router_g_w"], inp["router_e_w"]], axis=-1))
    sh["rb"] = np.ascontiguousarray(np.concatenate([inp["router_g_b"], inp["router_e_b"]], axis=-1)[:, None, :])
    for nm, key in (("wg", "exp_w_gate"), ("wu", "exp_w_up")):
        w = inp[key].reshape(2, NE, KC, 128, 2, 256)
        sh[nm] = np.ascontiguousarray(w.transpose(0, 1, 4, 3, 2, 5)).reshape(2, NE * 2 * 128, KC * 256)
    w = inp["exp_w_down"].reshape(2, NE, 4, 128, D)
    sh["wd"] = np.ascontiguousarray(w.transpose(0, 1, 3, 2, 4)).reshape(2, NE * 128, 4 * D)
    rows = S // GRID_W
    row = np.repeat(np.arange(rows), GRID_W).astype(np.float32); col = np.tile(np.arange(GRID_W), rows).astype(np.float32)
    inv = (10000.0 ** (-np.arange(0, 64, 2, dtype=np.float32) / 64)).astype(np.float32)
    ar = row[:, None] * inv; ac = col[:, None] * inv
    ang = np.concatenate([ar, ar, ac, ac], axis=-1)
    sgn = np.concatenate([-np.ones(32), np.ones(32), -np.ones(32), np.ones(32)]).astype(np.float32)
    pm = np.zeros((128, 128), np.float32)
    for m in range(128):
        pm[m + 32 if (m % 64) < 32 else m - 32, m] = 1.0
    sh["perm"] = pm
    sh["cs"] = np.ascontiguousarray(np.stack([np.cos(ang).T, (np.sin(ang) * sgn[None, :]).T]).astype(np.float32))
    return sh


def make_in_maps(inp, S, L, NB, ncores):
    inp = {k: np.asarray(v) for k, v in inp.items()}
    sh = prep_shared(inp, S)
    maps = []
    for ci in range(ncores):
        m = dict(sh)
        m["x"] = np.ascontiguousarray(inp["x"][ci * NB:(ci + 1) * NB])
        m["ctx"] = np.ascontiguousarray(inp["ctx"][ci * NB:(ci + 1) * NB])
        rows3 = np.stack([inp["c"][ci * NB + r] if r < NB else inp["c_ctx"] for r in range(3)]) if NB == 2 else None
        if NB == 1:
            rows3 = np.stack([inp["c"][ci], inp["c"][ci], inp["c_ctx"]])
        cT = rows3.reshape(3, KC, 128).transpose(2, 1, 0).reshape(128, KC * 3)
        m["cT"] = np.ascontiguousarray(cT)
        maps.append(m)
    return maps


_CACHE = {}


def kernel(**inputs):
    B, S, _ = inputs["x"].shape
    L = inputs["ctx"].shape[1]
    ncores = 8; NB = B // ncores
    key = (S, L, NB)
    if key not in _CACHE:
        _CACHE[key] = build(S, L, NB)
    nc = _CACHE[key]
    maps = make_in_maps(inputs, S, L, NB, ncores)
    res = run_bass_kernel_spmd(nc, maps, core_ids=list(range(ncores)))
    return np.concatenate([r["out"] for r in res.results], axis=0).astype(np.float32)
```

```python
import numpy as np
from contextlib import ExitStack
from functools import partial as P
import concourse.bass as bass
import concourse.mybir as mybir
from concourse.bass_utils import run_bass_kernel_spmd

F32 = mybir.dt.float32; BF16 = mybir.dt.bfloat16; U32 = mybir.dt.uint32; I32 = mybir.dt.int32
AF = mybir.ActivationFunctionType; ALU = mybir.AluOpType; AX = mybir.AxisListType
D = 2048; KC = 16; HD = 128; NH = 8; NKV = 2; NE = 32; DE = 512; EPS = 1e-6
GRID_W = 64
RELAX = True


class Tok:
    __slots__ = ("name", "w", "r", "rd", "hold")
    def __init__(s, name): s.name = name; s.w = None; s.r = {}; s.rd = []; s.hold = 0


class Op:
    __slots__ = ("eng", "fn", "deps", "signal", "sig", "dma", "key", "val")


class Sch:
    def __init__(s, nc, es):
        s.nc = nc; s.es = es; s.ops = []; s.cum = {}
        s.last = {}; s.lastdma = {}; s.pending = {}

    def barrier(s):
        b = set(s.last.values()) | set(s.lastdma.values())
        for e in ("pe", "act", "dve", "pool", "sp"):
            s.pending.setdefault(e, set()).update(b)

    def add(s, eng, fn, r=(), w=(), dma=False, key=None):
        op = Op(); op.eng = eng; op.fn = fn; op.dma = dma; op.signal = False; op.key = key; op.sig = 0; op.val = 0
        deps = set(); raw = set()
        for t in r:
            if t.w is not None: deps.add(t.w); raw.add(t.w)
        for t in w:
            if t.w is not None: deps.add(t.w)
            deps.update(t.r.values()); deps.update(t.rd)
        if eng in s.pending:
            pb = s.pending.pop(eng); deps.update(pb); raw.update(pb)
        fd = []
        for p in deps:
            if p is op: continue
            if (not p.dma) and (not dma) and p.eng == eng:
                if eng == "pe" or (RELAX and p not in raw): continue
            if not p.dma: p.signal = True
            fd.append(p)
        op.deps = fd
        if dma:
            assert key is not None
            s.cum[key] = s.cum.get(key, 0) + 16; op.val = s.cum[key]
        for t in r:
            if dma: t.rd.append(op)
            else: t.r[eng] = op
        for t in w:
            t.w = op; t.r = {}; t.rd = []
        if dma: s.lastdma[key] = op
        else: s.last[eng] = op
        s.ops.append(op)
        return op

    def emit(s):
        nc = s.nc
        eo = {"pe": nc.tensor, "act": nc.scalar, "dve": nc.vector, "pool": nc.gpsimd, "sp": nc.sync}
        esem = {e: s.es.enter_context(nc.semaphore("e_" + e)) for e in ("pe", "act", "dve", "pool")}
        dsem = {k: s.es.enter_context(nc.semaphore("d_%d" % i)) for i, k in enumerate(s.cum)}
        cnt = {e: 0 for e in esem}
        waited = {}
        for op in s.ops:
            need = {}
            for p in op.deps:
                if p.dma: k = ("d", p.key); v = p.val
                else: k = ("e", p.eng); v = p.sig
                if v > need.get(k, 0): need[k] = v
            for k, v in need.items():
                wk = (op.eng, k)
                if v > waited.get(wk, 0):
                    waited[wk] = v
                    eo[op.eng].wait_ge(dsem[k[1]] if k[0] == "d" else esem[k[1]], v)
            ins = op.fn()
            if op.dma:
                ins.then_inc(dsem[op.key], 16)
            elif op.signal:
                cnt[op.eng] += 1; op.sig = cnt[op.eng]
                ins.then_inc(esem[op.eng], 1)
        for k, v in s.cum.items():
            nc.sync.wait_ge(dsem[k], v)
        return len(dsem)


class Rot:
    def __init__(s, alloc, tk, name, shape, dt, n):
        s.bufs = [alloc("%s_%d" % (name, i), shape, dt) for i in range(n)]; s.toks = [tk(name) for _ in range(n)]; s.i = 0
    def next(s):
        i = s.i % len(s.bufs); s.i += 1
        return s.bufs[i], s.toks[i]


def pipeline(gens):
    active = []
    for g in gens:
        for a in list(active):
            try: next(a)
            except StopIteration: active.remove(a)
        active.append(g)
        try: next(g)
        except StopIteration: active.remove(g)
    while active:
        for a in list(active):
            try: next(a)
            except StopIteration: active.remove(a)


def build(S, L, NB, stop_after=None, dbg=False):
    NT = S + L; NTT = NT // 128; T = NB * NT
    assert S % 512 == 0 and L % 128 == 0 and L <= 512
    BLK = 256
    nc = bass.Bass("TRN2", target_bir_lowering=False)
    es = ExitStack()
    sch = Sch(nc, es)
    A = sch.add

    def dram(name, shape, dt=F32, kind="ExternalInput"):
        return nc.dram_tensor(name, shape, dt, kind=kind).ap()
    IK = "ExternalOutput" if dbg else "Internal"
    x_d = dram("x", [NB, S, D]); ctx_d = dram("ctx", [NB, L, D])
    cT_d = dram("cT", [128, KC * 3])
    wada_d = dram("w_ada", [2, D, 6 * D]); bada_d = dram("b_ada", [2, 1, 6 * D])
    NPV = 256
    pv_d = dram("pv", [2, 128, NPV])
    fg_d = dram("fg", [1, D])
    win_d = dram("w_in", [2, D, 4096]); wout_d = dram("w_out", [2, D, D])
    wr_d = dram("wr", [2, D, 36]); rb_d = dram("rb", [2, 1, 36])
    wg_d = dram("wg", [2, NE * 2 * 128, KC * 256]); wu_d = dram("wu", [2, NE * 2 * 128, KC * 256])
    wd_d = dram("wd", [2, NE * 128, 4 * D])
    cs_d = dram("cs", [2, 128, S])
    perm_d = dram("perm", [128, 128])
    UCd = dram("UCd", [NB, 4, 128, NT], F32, kind=IK)
    out_d = dram("out", [NB, S, D], kind="ExternalOutput")
    MOD = dram("MOD", [2, 3, 6 * D], kind=IK)
    QT = dram("QT", [NB, NH, 128, NT], BF16, kind=IK)
    KT = dram("KT", [NB, NKV, 128, NT], BF16, kind=IK)
    VV = dram("VV", [NB, NT, 256], BF16, kind=IK)
    YT = dram("YT", [NB, KC, 128, NT], F32, kind=IK)
    R = dram("R", [NB, NT, D], F32, kind=IK)
    H2 = dram("H2", [T, D], BF16, kind=IK)
    NBLK_MAX = (2 * T + BLK - 1) // BLK + NE
    XS = dram("XS", [NBLK_MAX * BLK, D], BF16, kind="Internal")
    YS = dram("YS", [NBLK_MAX * BLK, D], F32, kind="Internal")
    if dbg:
        dbg_d = dram("dbg", [NB, NT, D], kind="ExternalOutput")

    ucnt = [0]
    def uname(name):
        ucnt[0] += 1
        return "s%d_%s" % (ucnt[0], name)
    def sb(name, shape, dt=F32):
        return es.enter_context(nc.sbuf_tensor(uname(name), shape, dt))
    tokc = [0]
    def tk(name="t"):
        tokc[0] += 1
        return Tok("%s%d" % (name, tokc[0]))

    PS = [es.enter_context(nc.psum_tensor("ps%d" % i, [128, 512], F32)) for i in range(8)]
    PT = [tk("ps") for _ in range(8)]
    psrr = [0]
    def psnext():
        for _ in range(8):
            i = psrr[0] % 8; psrr[0] += 1
            t = PT[i]
            if getattr(t, "hold", 0) == 0 and (t.w is None or t.r or t.rd):
                return PS[i], t
        raise RuntimeError("PSUM: no free bank (too many live tiles in the pipeline)")

    ident = sb("ident", [128, 128]); t_ident = tk()
    identb = sb("identb", [128, 128], BF16); t_identb = tk()
    onesb = sb("onesb", [128, 128], BF16); t_onesb = tk()
    onesf = sb("onesf", [128, 128]); t_onesf = tk()
    perm = sb("perm", [128, 128]); t_perm = tk()
    ustr = sb("ustr", [128, 128], BF16); t_ustr = tk()
    iot = sb("iot", [128, 128]); t_iot = tk()
    pidx = sb("pidx", [128, 1]); t_pidx = tk()
    eps_t = sb("eps_t", [128, 1]); t_eps = tk()
    iot32 = sb("iot32", [128, 32]); t_iot32 = tk()
    A("pool", P(nc.gpsimd.iota, iot[:], pattern=[[1, 128]], base=0, channel_multiplier=0, allow_small_or_imprecise_dtypes=True), w=[t_iot])
    A("pool", P(nc.gpsimd.iota, pidx[:], pattern=[[0, 1]], base=0, channel_multiplier=1, allow_small_or_imprecise_dtypes=True), w=[t_pidx])
    A("dve", P(nc.vector.memset, eps_t[:], EPS), w=[t_eps])
    A("dve", P(nc.vector.memset, onesb[:], 1.0), w=[t_onesb])
    A("dve", P(nc.vector.memset, onesf[:], 1.0), w=[t_onesf])
    A("dve", P(nc.vector.tensor_copy, out=iot32[:], in_=iot[:, 0:32]), r=[t_iot], w=[t_iot32])
    A("dve", P(nc.vector.tensor_scalar, out=ident[:], in0=iot[:], scalar1=pidx[:, 0:1], scalar2=None, op0=ALU.is_equal), r=[t_iot, t_pidx], w=[t_ident])
    A("dve", P(nc.vector.tensor_copy, out=identb[:], in_=ident[:]), r=[t_ident], w=[t_identb])
    A("dve", P(nc.vector.tensor_scalar, out=ustr[:], in0=iot[:], scalar1=pidx[:, 0:1], scalar2=None, op0=ALU.is_gt), r=[t_iot, t_pidx], w=[t_ustr])
    A("sp", P(nc.sync.dma_start, out=perm[:], in_=perm_d), w=[t_perm], dma=True, key="perm")

    cosT = sb("cosT", [128, S]); sinT = sb("sinT", [128, S]); t_cs = tk()
    A("sp", P(nc.sync.dma_start, out=cosT[:], in_=cs_d[0]), w=[t_cs], dma=True, key="cs")
    A("sp", P(nc.sync.dma_start, out=sinT[:], in_=cs_d[1]), w=[t_cs], dma=True, key="cs")
    pv = [sb("pv%d" % l, [128, NPV]) for l in range(2)]; t_pv = [tk() for _ in range(2)]
    for l in range(2):
        A("sp", (P(nc.sync.dma_start, out=pv[l][:], in_=pv_d[l])), w=[t_pv[l]], dma=True, key="pv%d" % l)
    PV_G1, PV_G2, PV_GG, PV_QG, PV_KG, PV_SW, PV_CW, PV_CB, PV_LG, PV_LB = 0, 16, 32, 48, 49, 50, 62, 186, 190, 194

    csil = sb("csil", [128, KC * 3]); t_csil = tk()
    A("sp", P(nc.sync.dma_start, out=csil[:], in_=cT_d), w=[t_csil], dma=True, key="csil")
    A("act", P(nc.scalar.activation, out=csil[:], in_=csil[:], func=AF.Silu), r=[t_csil], w=[t_csil])
    modT = [sb("modT%d" % l, [128, 96 * 3]) for l in range(2)]; t_modT = [tk() for _ in range(2)]
    with ExitStack() as ph:
        sch.barrier()
        def psb(name, shape, dt=F32):
            return ph.enter_context(nc.sbuf_tensor(uname(name), shape, dt))
        wa = [psb("wa%d" % i, [128, KC, 512]) for i in range(2)]; t_wa = [tk() for _ in range(2)]
        mrow = psb("mrow", [3, 6 * D]); t_mrow = tk()
        brows = [psb("brow%d" % i, [3, 512]) for i in range(2)]; t_brows = [tk() for _ in range(2)]
        t_MODs = [tk("MOD") for _ in range(2)]
        for l in range(2):
            for nb in range(24):
                i = nb % 2
                brow = brows[i]; t_brow = t_brows[i]
                A("sp", (P(nc.sync.dma_start, out=brow[:], in_=bada_d[l, 0, nb * 512:(nb + 1) * 512].partition_broadcast(3))), w=[t_brow], dma=True, key="brow%d" % i)
                A("sp", (P(nc.sync.dma_start, out=wa[i][:], in_=wada_d[l, :, nb * 512:(nb + 1) * 512].rearrange("(c p) n -> p c n", p=128))), w=[t_wa[i]], dma=True, key="wa%d" % i)
                pt, ptk = psnext()
                for c in range(KC):
                    A("pe", (P(nc.tensor.matmul, pt[0:3, :], lhsT=csil[:, c * 3:(c + 1) * 3], rhs=wa[i][:, c, :], start=(c == 0), stop=(c == KC - 1))), r=[t_csil, t_wa[i]], w=[ptk])
                A("dve", (P(nc.vector.tensor_tensor, out=mrow[:, nb * 512:(nb + 1) * 512], in0=pt[0:3, :], in1=brow[:, :], op=ALU.add)), r=[ptk, t_brow], w=[t_mrow])
            A("sp", (P(nc.sync.dma_start, out=MOD[l], in_=mrow[:])), r=[t_mrow], w=[t_MODs[l]], dma=True, key="mrow")
            for j0 in range(0, 96, 32):
                pt, ptk = psnext()
                for j in range(j0, j0 + 32):
                    A("pe", (P(nc.tensor.transpose, pt[:, (j - j0) * 3:(j - j0 + 1) * 3], mrow[0:3, j * 128:(j + 1) * 128], ident[0:3, 0:3])), r=[t_mrow, t_ident], w=[ptk])
                A("dve", (P(nc.vector.tensor_copy, out=modT[l][:, j0 * 3:(j0 + 32) * 3], in_=pt[:, 0:96])), r=[ptk], w=[t_modT[l]])
    t_MODd = tk("MOD")

    def modv(l, sec, r3):
        return modT[l][:, sec * 48:(sec + 1) * 48].rearrange("p (c r) -> p c r", r=3)[:, :, r3]

    Avec = [[sb("Av%d_%d" % (l, k), [128, 3 * KC]) for k in range(2)] for l in range(2)]
    t_Av = [tk() for _ in range(2)]
    for l in range(2):
        for k, (sec, gcol) in enumerate(((1, PV_G1), (4, PV_G2))):
            for r3 in range(3):
                A("dve", (P(nc.vector.scalar_tensor_tensor,
                    out=Avec[l][k][:, r3 * KC:(r3 + 1) * KC], in0=modv(l, sec, r3), scalar=1.0, in1=pv[l][:, gcol:gcol + KC], op0=ALU.add, op1=ALU.mult)),
                  r=[t_modT[l], t_pv[l]], w=[t_Av[l]])

    state = {"rtok": [[tk("R") for _ in range(NTT)] for _ in range(NB)]}

    def resid_src(l, b, tt):
        if l == 0:
            if tt < S // 128: return x_d[b, tt * 128:(tt + 1) * 128, :]
            return ctx_d[b, tt * 128 - S:(tt + 1) * 128 - S, :]
        return R[b, tt * 128:(tt + 1) * 128, :]

    tgs = [(g * 512, 512) for g in range(S // 512)] + [(S, L)]

    for l in range(2):
        last = (l == 1)
        t_QT = [tk("QT") for _ in range(NB)]; t_KT = [tk("KT") for _ in range(NB)]; t_VV = [tk("VV") for _ in range(NB)]
        t_YT = [[tk("YT") for _ in range(KC)] for _ in range(NB)]
        for b in range(NB):
            with ExitStack() as ph:
                sch.barrier()
                def psb(name, shape, dt=F32):
                    return ph.enter_context(nc.sbuf_tensor(uname(name), shape, dt))
                hT = psb("hT", [128, KC, NT], BF16); t_hT = [[tk("hTlo"), tk("hThi")] for _ in range(NTT)]
                with ExitStack() as ph1:
                    sch.barrier()
                    def psb1(name, shape, dt=F32):
                        return ph1.enter_context(nc.sbuf_tensor(uname(name), shape, dt))
                    xtR = Rot(psb1, tk, "xt", [128, D], F32, 4)
                    xnR = Rot(psb1, tk, "xn", [128, D], F32, 3)
                    junkR = Rot(psb1, tk, "junk", [128, D], BF16, 2)
                    stR = Rot(psb1, tk, "st", [128, 4], F32, 6)

                    def b1_item(tt):
                        r3 = b if tt < S // 128 else 2
                        xt_, t_xt_ = xtR.next(); st, t_st = stR.next()
                        A("sp", (P(nc.sync.dma_start, out=xt_[:], in_=resid_src(l, b, tt))), r=[state["rtok"][b][tt]], w=[t_xt_], dma=True, key="xt%d" % (xtR.i % 4))
                        yield
                        junk, t_junk = junkR.next()
                        A("act", (P(nc.scalar.activation, out=junk[:], in_=xt_[:], func=AF.Square, accum_out=st[:, 0:1])), r=[t_xt_], w=[t_junk, t_st])
                        A("act", (P(nc.scalar.activation, out=st[:, 1:2], in_=st[:, 0:1], func=AF.Sqrt, scale=1.0 / D, bias=eps_t[:, 0:1])), r=[t_st, t_eps], w=[t_st])
                        A("dve", (P(nc.vector.reciprocal, out=st[:, 2:3], in_=st[:, 1:2])), r=[t_st], w=[t_st])
                        yield
                        xn_, t_xn_ = xnR.next()
                        A("act", (P(nc.scalar.activation, out=xn_[:], in_=xt_[:], func=AF.Identity, scale=st[:, 2:3])), r=[t_xt_, t_st], w=[t_xn_])
                        yield
                        pts = []
                        for c0 in range(0, KC, 4):
                            pt, ptk = psnext(); pts.append((pt, ptk))
                            for c in range(c0, c0 + 4):
                                A("pe", (P(nc.tensor.transpose, pt[:, (c - c0) * 128:(c - c0 + 1) * 128], xn_[:, c * 128:(c + 1) * 128], ident[:])), r=[t_xn_, t_ident], w=[ptk])
                        yield
                        for c0 in range(0, KC, 4):
                            pt, ptk = pts[c0 // 4]
                            for c in range(c0, c0 + 4):
                                sc_ap = Avec[l][0][:, r3 * KC + c: r3 * KC + c + 1]
                                bi_ap = modT[l][:, (0 * 16 + c) * 3 + r3:(0 * 16 + c) * 3 + r3 + 1]
                                if c < 8:
                                    A("dve", (P(nc.vector.tensor_scalar, out=hT[:, c, tt * 128:(tt + 1) * 128], in0=pt[:, (c - c0) * 128:(c - c0 + 1) * 128], scalar1=sc_ap, scalar2=bi_ap, op0=ALU.mult, op1=ALU.add)),
                                      r=[ptk, t_Av[l], t_modT[l]], w=[t_hT[tt][c // 8]])
                                else:
                                    A("act", (P(nc.scalar.activation, out=hT[:, c, tt * 128:(tt + 1) * 128], in_=pt[:, (c - c0) * 128:(c - c0 + 1) * 128], func=AF.Identity, scale=sc_ap, bias=bi_ap)),
                                      r=[ptk, t_Av[l], t_modT[l]], w=[t_hT[tt][c // 8]])
                    pipeline(b1_item(tt) for tt in range(NTT))
                sch.barrier()
                wt = [psb("wt%d" % i, [128, KC, 512], BF16) for i in range(2)]; t_wt = [tk() for _ in range(2)]
                SEQW = 16 + S + 16 + L + 16
                OFFC = 16 + S + 16
                def seqoff(t0):
                    return 16 + t0 if t0 < S else OFFC + (t0 - S)
                sqR = Rot(psb, tk, "sq", [128, 512], BF16, 2)
                rsR = Rot(psb, tk, "rs", [128, 512], F32, 2)
                qnR = Rot(psb, tk, "qn", [128, 512], F32, 2)
                t1R = Rot(psb, tk, "t1", [128, 512], F32, 2)
                t2R = Rot(psb, tk, "t2", [128, 512], F32, 2)
                qoR = Rot(psb, tk, "qo", [128, 512], BF16, 3)
                vo = [psb("vo%d" % i, [128, 256], BF16) for i in range(2)]; t_vo = [tk() for _ in range(2)]
                wgi = [0]
                def load_w(col0, ncol):
                    i = wgi[0] % 2; wgi[0] += 1
                    A("pool", (P(nc.gpsimd.dma_start, out=wt[i][:, :, 0:ncol], in_=win_d[l, :, col0:col0 + ncol].rearrange("(c p) n -> p c n", p=128))), w=[t_wt[i]], dma=True, key="wt%d" % i)
                    return i

                def proj(i, j, t0, n, pt, ptk):
                    for c in range(KC):
                        tts = [t_hT[t][c // 8] for t in range(t0 // 128, (t0 + n) // 128)]
                        A("pe", (P(nc.tensor.matmul, pt[:, 0:n], lhsT=wt[i][:, c, j * 128:(j + 1) * 128], rhs=hT[:, c, t0:t0 + n], start=(c == 0), stop=(c == KC - 1))), r=[t_wt[i]] + tts, w=[ptk])

                def qk_item(i, j, gcol, dst, t_dst, t0, n):
                    pt, ptk = psnext(); ptk.hold = 1
                    proj(i, j, t0, n, pt, ptk)
                    sq, t_sq = sqR.next()
                    A("act", (P(nc.scalar.activation, out=sq[:, 0:n], in_=pt[:, 0:n], func=AF.Square)), r=[ptk], w=[t_sq])
                    yield
                    p2, p2k = psnext()
                    rs, t_rs = rsR.next()
                    A("pe", (P(nc.tensor.matmul, p2[:, 0:n], lhsT=onesb[:], rhs=sq[:, 0:n], start=True, stop=True)), r=[t_sq, t_onesb], w=[p2k])
                    A("act", (P(nc.scalar.activation, out=rs[:, 0:n], in_=p2[:, 0:n], func=AF.Sqrt, scale=1.0 / HD, bias=eps_t[:, 0:1])), r=[p2k, t_eps], w=[t_rs])
                    A("dve", (P(nc.vector.reciprocal, out=rs[:, 0:n], in_=rs[:, 0:n])), r=[t_rs], w=[t_rs])
                    qo_, t_qo_ = qoR.next(); qkey = "qo%d" % (qoR.i % 3)
                    if t0 >= S:
                        A("dve", (P(nc.vector.scalar_tensor_tensor, out=qo_[:, 0:n], in0=pt[:, 0:n], scalar=pv[l][:, gcol:gcol + 1], in1=rs[:, 0:n], op0=ALU.mult, op1=ALU.mult)), r=[ptk, t_rs, t_pv[l]], w=[t_qo_])
                        ptk.hold = 0
                    else:
                        qn, t_qn = qnR.next(); t1, t_t1 = t1R.next(); t2, t_t2 = t2R.next()
                        A("dve", (P(nc.vector.scalar_tensor_tensor, out=qn[:, 0:n], in0=pt[:, 0:n], scalar=pv[l][:, gcol:gcol + 1], in1=rs[:, 0:n], op0=ALU.mult, op1=ALU.mult)), r=[ptk, t_rs, t_pv[l]], w=[t_qn])
                        ptk.hold = 0
                        A("pool", (P(nc.gpsimd.tensor_tensor, out=t1[:, 0:n], in0=qn[:, 0:n], in1=cosT[:, t0:t0 + n], op=ALU.mult)), r=[t_qn, t_cs], w=[t_t1])
                        yield
                        p3, p3k = psnext()
                        A("pe", (P(nc.tensor.matmul, p3[:, 0:n], lhsT=perm[:], rhs=qn[:, 0:n], start=True, stop=True)), r=[t_qn, t_perm], w=[p3k])
                        A("dve", (P(nc.vector.tensor_tensor, out=t2[:, 0:n], in0=p3[:, 0:n], in1=sinT[:, t0:t0 + n], op=ALU.mult)), r=[p3k, t_cs], w=[t_t2])
                        A("dve", (P(nc.vector.tensor_tensor, out=qo_[:, 0:n], in0=t1[:, 0:n], in1=t2[:, 0:n], op=ALU.add)), r=[t_t1, t_t2], w=[t_qo_])
                    A("sp", (P(nc.sync.dma_start, out=dst[:, t0:t0 + n], in_=qo_[:, 0:n])), r=[t_qo_], w=[t_dst], dma=True, key=qkey)

                def qk_chunk(i, j, gcol, dst, t_dst):
                    return [qk_item(i, j, gcol, dst, t_dst, t0, n) for (t0, n) in tgs]

                for g in range(2):
                    i = load_w(g * 512, 512)
                    items = []
                    for j in range(4):
                        items += qk_chunk(i, j, PV_QG, QT[b, g * 4 + j], t_QT[b])
                    pipeline(items)
                i = load_w(1024, 512)
                items = []
                for j in range(2):
                    items += qk_chunk(i, j, PV_KG, KT[b, j], t_KT[b])
                pipeline(items)
                for tt in range(NTT):
                    pt, ptk = psnext()
                    for c in range(KC):
                        A("pe", (P(nc.tensor.matmul, pt[:, 0:256], lhsT=hT[:, c, tt * 128:(tt + 1) * 128], rhs=wt[i][:, c, 256:512], start=(c == 0), stop=(c == KC - 1))), r=[t_wt[i], t_hT[tt][c // 8]], w=[ptk])
                    vi = tt % 2
                    A("act", (P(nc.scalar.activation, func=AF.Copy, out=vo[vi][:], in_=pt[:, 0:256])), r=[ptk], w=[t_vo[vi]])
                    A("sp", (P(nc.sync.dma_start, out=VV[b, tt * 128:(tt + 1) * 128, :], in_=vo[vi][:])), r=[t_vo[vi]], w=[t_VV[b]], dma=True, key="vo%d" % vi)

                seqA = psb("seqA", [128, SEQW], BF16); t_seqA = tk()
                gbuf = psb("gbuf", [128, NT]); t_gbuf = tk()
                tmpfR = Rot(psb, tk, "tmpf", [128, 512], F32, 2)
                dg = psb("dg", [128, 31, 128], BF16); t_dg = tk()
                yo = [psb("yo%d" % i2, [128, 512]) for i2 in range(2)]; t_yo = [tk() for _ in range(2)]
                t_UC = [tk("UC") for _ in range(4)]
                A("dve", P(nc.vector.memset, seqA[:], 0.0), w=[t_seqA])
                yoc = [0]

                def build_diag(dgt, t_dgt, ntap, col0, stride, j):
                    for k in range(ntap):
                        A("dve", (P(nc.vector.tensor_scalar, out=dgt[:, k, :], in0=ident[:], scalar1=pv[l][:, col0 + k * stride + j: col0 + k * stride + j + 1], scalar2=None, op0=ALU.mult)), r=[t_ident, t_pv[l]], w=[t_dgt])

                def dwconv(dgt, t_dgt, seq, t_seq, ntap, t0, n, pt, ptk):
                    half = (ntap - 1) // 2
                    o = seqoff(t0)
                    for k in range(ntap):
                        A("pe", (P(nc.tensor.matmul, pt[:, 0:n], lhsT=dgt[:, k, :], rhs=seq[:, o + k - half: o + k - half + n], start=(k == 0), stop=(k == ntap - 1))), r=[t_dgt, t_seq], w=[ptk])

                for j in range(4):
                    i = load_w(1536 + j * 384, 384)
                    build_diag(dg, t_dg, 3, PV_SW, 4, j)
                    for (t0, n) in tgs:
                        pb, pbk = psnext(); proj(i, 0, t0, n, pb, pbk)
                        pc, pck = psnext(); proj(i, 1, t0, n, pc, pck)
                        pu, puk = psnext(); proj(i, 2, t0, n, pu, puk)
                        A("act", (P(nc.scalar.activation, func=AF.Copy, out=gbuf[:, t0:t0 + n], in_=pb[:, 0:n])), r=[pbk], w=[t_gbuf])
                        tmpf, t_tmpf = tmpfR.next()
                        A("act", (P(nc.scalar.activation, func=AF.Copy, out=tmpf[:, 0:n], in_=pc[:, 0:n])), r=[pck], w=[t_tmpf])
                        A("dve", (P(nc.vector.tensor_tensor, out=seqA[:, seqoff(t0):seqoff(t0) + n], in0=tmpf[:, 0:n], in1=pu[:, 0:n], op=ALU.mult)), r=[t_tmpf, puk], w=[t_seqA])
                    for (t0, n) in tgs:
                        pt, ptk = psnext()
                        dwconv(dg, t_dg, seqA, t_seqA, 3, t0, n, pt, ptk)
                        yi = yoc[0] % 2; yoc[0] += 1
                        A("dve", (P(nc.vector.tensor_tensor, out=yo[yi][:, 0:n], in0=pt[:, 0:n], in1=gbuf[:, t0:t0 + n], op=ALU.mult)), r=[ptk, t_gbuf], w=[t_yo[yi]])
                        A("sp", (P(nc.sync.dma_start, out=YT[b, 8 + j, :, t0:t0 + n], in_=yo[yi][:, 0:n])), r=[t_yo[yi]], w=[t_YT[b][8 + j]], dma=True, key="yo%d" % yi)
                for g in range(2):
                    i = load_w(3072 + g * 512, 512)
                    for jj in range(2):
                        j = 2 * g + jj
                        build_diag(dg, t_dg, 31, PV_CW, 4, j)
                        for (t0, n) in tgs:
                            pa, pak = psnext(); proj(i, 2 * jj, t0, n, pa, pak)
                            pg, pgk = psnext(); proj(i, 2 * jj + 1, t0, n, pg, pgk)
                            tmpf, t_tmpf = tmpfR.next()
                            A("act", (P(nc.scalar.activation, out=tmpf[:, 0:n], in_=pg[:, 0:n], func=AF.Sigmoid)), r=[pgk], w=[t_tmpf])
                            A("dve", (P(nc.vector.tensor_tensor, out=seqA[:, seqoff(t0):seqoff(t0) + n], in0=tmpf[:, 0:n], in1=pa[:, 0:n], op=ALU.mult)), r=[t_tmpf, pak], w=[t_seqA])
                        for (t0, n) in tgs:
                            pt, ptk = psnext()
                            dwconv(dg, t_dg, seqA, t_seqA, 31, t0, n, pt, ptk)
                            yi = yoc[0] % 2; yoc[0] += 1
                            A("act", (P(nc.scalar.activation, out=yo[yi][:, 0:n], in_=pt[:, 0:n], func=AF.Identity, bias=pv[l][:, PV_CB + j:PV_CB + j + 1])), r=[ptk, t_pv[l]], w=[t_yo[yi]])
                            A("sp", (P(nc.sync.dma_start, out=UCd[b, j, :, t0:t0 + n], in_=yo[yi][:, 0:n])), r=[t_yo[yi]], w=[t_UC[j]], dma=True, key="yo%d" % yi)
            with ExitStack() as ph:
                sch.barrier()
                def psb(name, shape, dt=F32):
                    return ph.enter_context(nc.sbuf_tensor(uname(name), shape, dt))
                ucl = [psb("ucl%d" % i2, [128, 4, 512]) for i2 in range(2)]; t_ucl = [tk() for _ in range(2)]
                mean = psb("mean", [128, 512]); t_mean = tk()
                var = psb("var", [128, 512]); t_var = tk()
                tmpfR = Rot(psb, tk, "tmpf3", [128, 512], F32, 2)
                usqR = Rot(psb, tk, "usq", [128, 512], F32, 2)
                yo = [psb("yo3_%d" % i2, [128, 512]) for i2 in range(2)]; t_yo = [tk() for _ in range(2)]
                yoc = [0]
                for gi_, (t0, n) in enumerate(tgs):
                    ui = gi_ % 2; uc = ucl[ui]
                    for j in range(4):
                        A("sp", (P(nc.sync.dma_start, out=uc[:, j, 0:n], in_=UCd[b, j, :, t0:t0 + n])), r=[t_UC[j]], w=[t_ucl[ui]], dma=True, key="ucl%d" % ui)
                    p1, p1k = psnext(); p2, p2k = psnext()
                    for j in range(4):
                        A("pe", (P(nc.tensor.matmul, p1[:, 0:n], lhsT=onesf[:], rhs=uc[:, j, 0:n], start=(j == 0), stop=(j == 3))), r=[t_ucl[ui], t_onesf], w=[p1k])
                    for j in range(4):
                        usq, t_usq = usqR.next()
                        A("pool", (P(nc.gpsimd.tensor_tensor, out=usq[:, 0:n], in0=uc[:, j, 0:n], in1=uc[:, j, 0:n], op=ALU.mult)), r=[t_ucl[ui]], w=[t_usq])
                        A("pe", (P(nc.tensor.matmul, p2[:, 0:n], lhsT=onesf[:], rhs=usq[:, 0:n], start=(j == 0), stop=(j == 3))), r=[t_usq, t_onesf], w=[p2k])
                    A("act", (P(nc.scalar.activation, out=mean[:, 0:n], in_=p1[:, 0:n], func=AF.Copy, scale=1.0 / 512)), r=[p1k], w=[t_mean])
                    A("dve", (P(nc.vector.tensor_tensor, out=var[:, 0:n], in0=mean[:, 0:n], in1=mean[:, 0:n], op=ALU.mult)), r=[t_mean], w=[t_var])
                    A("dve", (P(nc.vector.scalar_tensor_tensor, out=var[:, 0:n], in0=p2[:, 0:n], scalar=1.0 / 512, in1=var[:, 0:n], op0=ALU.mult, op1=ALU.subtract)), r=[p2k, t_var], w=[t_var])
                    A("act", (P(nc.scalar.activation, out=var[:, 0:n], in_=var[:, 0:n], func=AF.Sqrt, bias=eps_t[:, 0:1])), r=[t_var, t_eps], w=[t_var])
                    A("dve", (P(nc.vector.reciprocal, out=var[:, 0:n], in_=var[:, 0:n])), r=[t_var], w=[t_var])
                    for j in range(4):
                        tmpf, t_tmpf = tmpfR.next()
                        A("dve", (P(nc.vector.tensor_tensor, out=tmpf[:, 0:n], in0=uc[:, j, 0:n], in1=mean[:, 0:n], op=ALU.subtract)), r=[t_ucl[ui], t_mean], w=[t_tmpf])
                        A("dve", (P(nc.vector.tensor_tensor, out=tmpf[:, 0:n], in0=tmpf[:, 0:n], in1=var[:, 0:n], op=ALU.mult)), r=[t_tmpf, t_var], w=[t_tmpf])
                        yi = yoc[0] % 2; yoc[0] += 1
                        A("act", (P(nc.scalar.activation, out=yo[yi][:, 0:n], in_=tmpf[:, 0:n], func=AF.Silu, scale=pv[l][:, PV_LG + j:PV_LG + j + 1], bias=pv[l][:, PV_LB + j:PV_LB + j + 1])), r=[t_tmpf, t_pv[l]], w=[t_yo[yi]])
                        A("sp", (P(nc.sync.dma_start, out=YT[b, 12 + j, :, t0:t0 + n], in_=yo[yi][:, 0:n])), r=[t_yo[yi]], w=[t_YT[b][12 + j]], dma=True, key="yo3_%d" % yi)

        SCALE = HD ** -0.5
        for b in range(NB):
            with ExitStack() as ph:
                sch.barrier()
                def psb(name, shape, dt=F32):
                    return ph.enter_context(nc.sbuf_tensor(uname(name), shape, dt))
                kt = [psb("kt%d" % i, [128, NT], BF16) for i in range(2)]; t_kt = [tk() for _ in range(2)]
                vt = [psb("vt%d" % i, [128, NTT, 128], BF16) for i in range(2)]; t_vt = [tk() for _ in range(2)]
                for kv in range(NKV):
                    A("sp", (P(nc.sync.dma_start, out=kt[kv][:], in_=KT[b, kv])), r=[t_KT[b]], w=[t_kt[kv]], dma=True, key="kt%d" % kv)
                    A("sp", (P(nc.sync.dma_start, out=vt[kv][:], in_=VV[b, :, kv * 128:(kv + 1) * 128].rearrange("(t p) d -> p t d", p=128))), r=[t_VV[b]], w=[t_vt[kv]], dma=True, key="vt%d" % kv)
                qc = [0]
                qgroups = [(g * 512, 512, 0, NTT) for g in range(S // 512)]
                if not last:
                    qgroups.append((S, L, S // 128, NTT))
                pTR = Rot(psb, tk, "pTr", [128, 512], BF16, 4)
                sbank = [0]

                rdenR = Rot(psb, tk, "rdenr", [128, 512], F32, 2)
                ooR = Rot(psb, tk, "oor", [128, 512], F32, 2)
                qtR = Rot(psb, tk, "qtr", [128, 512], BF16, 3)
                def allitems2():
                    for h in range(NH):
                        for (t0, n, k0, k1) in qgroups:
                            ab = (qc[0] % 2) * 2; qc[0] += 1
                            kv = h // (NH // NKV)
                            qt_, t_qt_ = qtR.next(); qkey = "qt%d" % (qtR.i % 3)
                            A("sp", (P(nc.sync.dma_start, out=qt_[:, 0:n], in_=QT[b, h, :, t0:t0 + n])), r=[t_QT[b]], w=[t_qt_], dma=True, key=qkey)
                            po, pok = PS[ab], PT[ab]; pd, pdk = PS[ab + 1], PT[ab + 1]
                            for kt_i in range(k0, k1):
                                yield kt_item2(po, pok, pd, pdk, kv, kt_i, qt_, t_qt_, n, k0, k1, h, t0)

                def kt_item2(po, pok, pd, pdk, kv, kt_i, qt_, t_qt_, n, k0, k1, h, t0):
                    si = 4 + (sbank[0] % 4); sbank[0] += 1
                    ps_, psk = PS[si], PT[si]
                    pT_, t_pT_ = pTR.next()
                    A("pe", (P(nc.tensor.matmul, ps_[:, 0:n], lhsT=kt[kv][:, kt_i * 128:(kt_i + 1) * 128], rhs=qt_[:, 0:n], start=True, stop=True)), r=[t_kt[kv], t_qt_], w=[psk])
                    A("act", (P(nc.scalar.activation, out=pT_[:, 0:n], in_=ps_[:, 0:n], func=AF.Exp, scale=SCALE)), r=[psk], w=[t_pT_])
                    yield
                    yield
                    A("pe", (P(nc.tensor.matmul, po[:, 0:n], lhsT=vt[kv][:, kt_i, :], rhs=pT_[:, 0:n], start=(kt_i == k0), stop=(kt_i == k1 - 1))), r=[t_vt[kv], t_pT_], w=[pok])
                    A("pe", (P(nc.tensor.matmul, pd[:, 0:n], lhsT=onesb[:], rhs=pT_[:, 0:n], start=(kt_i == k0), stop=(kt_i == k1 - 1))), r=[t_onesb, t_pT_], w=[pdk])
                    if kt_i == k1 - 1:
                        rden, t_rden = rdenR.next(); oo_, t_oo_ = ooR.next(); okey = "oo%d" % (ooR.i % 2)
                        A("dve", (P(nc.vector.reciprocal, out=rden[:, 0:n], in_=pd[:, 0:n])), r=[pdk], w=[t_rden])
                        A("dve", (P(nc.vector.tensor_tensor, out=oo_[:, 0:n], in0=po[:, 0:n], in1=rden[:, 0:n], op=ALU.mult)), r=[pok, t_rden], w=[t_oo_])
                        A("sp", (P(nc.sync.dma_start, out=YT[b, h, :, t0:t0 + n], in_=oo_[:, 0:n])), r=[t_oo_], w=[t_YT[b][h]], dma=True, key=okey)
                pipeline(allitems2())

        ntok_moe = NT if not last else S
        NTM = ntok_moe // 128
        TM = NB * ntok_moe
        NBLK = (2 * TM + BLK - 1) // BLK + NE
        t_H2 = tk("H2")
        with ExitStack() as phm:
            sch.barrier()
            def psbm(name, shape, dt=F32):
                return phm.enter_context(nc.sbuf_tensor(uname(name), shape, dt))
            RW = psbm("RW", [128, NB * NTM, 2]); t_RW = tk()
            M1 = psbm("M1", [128, NB * NTM, 32]); M2 = psbm("M2", [128, NB * NTM, 32]); t_M = tk()
            RK = psbm("RK", [128, NB * NTM, 2]); t_RK = tk()
            macc = psbm("macc", [128, 32]); t_macc = tk()
            A("dve", P(nc.vector.memset, macc[:], 0.0), w=[t_macc])
            with ExitStack() as ph:
                sch.barrier()
                def psb(name, shape, dt=F32):
                    return ph.enter_context(nc.sbuf_tensor(uname(name), shape, dt))
                wo = psb("wo", [128, KC, D], BF16); t_wo = tk()
                for c0 in range(0, KC, 4):
                    A("pool", (P(nc.gpsimd.dma_start, out=wo[:, c0:c0 + 4, :], in_=wout_d[l, c0 * 128:(c0 + 4) * 128, :].rearrange("(c p) n -> p c n", p=128))), w=[t_wo], dma=True, key="wo")
                gbcs = {}
                ycR = Rot(psb, tk, "yc", [128, 512], F32, 4)
                ysqR = Rot(psb, tk, "ysq", [128, 512], BF16, 3)
                rgR = Rot(psb, tk, "rg", [128, 3, 512], F32, 2)
                ynbR = Rot(psb, tk, "ynb", [128, KC, 512], BF16, 2)
                xrR = Rot(psb, tk, "xr", [128, D], F32, 2)
                h2R = Rot(psb, tk, "h2d", [128, 512], F32, 3)
                groups3 = [(0, 8), (8, 12), (12, 16)]

                def ld_chunk(b, c, t0, n):
                    yc, t_yc = ycR.next(); key = "yc%d" % (ycR.i % 4)
                    A("sp", (P(nc.sync.dma_start, out=yc[:, 0:n], in_=YT[b, c, :, t0:t0 + n])), r=[t_YT[b][c]], w=[t_yc], dma=True, key=key)
                    return yc, t_yc

                def d1_item(b, t0, n, gbc, t_gbc):
                    rg, t_rg = rgR.next(); ynb, t_ynb = ynbR.next()
                    for gi, (c0, c1) in enumerate(groups3):
                        pt, ptk = psnext()
                        for c in range(c0, c1):
                            yc, t_yc = ld_chunk(b, c, t0, n)
                            ysq, t_ysq = ysqR.next()
                            A("act", (P(nc.scalar.activation, out=ysq[:, 0:n], in_=yc[:, 0:n], func=AF.Square)), r=[t_yc], w=[t_ysq])
                            A("pe", (P(nc.tensor.matmul, pt[:, 0:n], lhsT=onesb[:], rhs=ysq[:, 0:n], start=(c == c0), stop=(c == c1 - 1))), r=[t_ysq, t_onesb], w=[ptk])
                        A("act", (P(nc.scalar.activation, out=rg[:, gi, 0:n], in_=pt[:, 0:n], func=AF.Sqrt, scale=1.0 / (128 * (c1 - c0)), bias=eps_t[:, 0:1])), r=[ptk, t_eps], w=[t_rg])
                        A("dve", (P(nc.vector.reciprocal, out=rg[:, gi, 0:n], in_=rg[:, gi, 0:n])), r=[t_rg], w=[t_rg])
                    for gi, (c0, c1) in enumerate(groups3):
                        for c in range(c0, c1):
                            yc, t_yc = ld_chunk(b, c, t0, n)
                            A("dve", (P(nc.vector.scalar_tensor_tensor, out=ynb[:, c, 0:n], in0=yc[:, 0:n], scalar=pv[l][:, PV_GG + c:PV_GG + c + 1], in1=rg[:, gi, 0:n], op0=ALU.mult, op1=ALU.mult)), r=[t_yc, t_rg, t_pv[l]], w=[t_ynb])
                    yield
                    for tj in range(n // 128):
                        tt = t0 // 128 + tj
                        xr, t_xr = xrR.next(); xkey = "xr%d" % (xrR.i % 2)
                        A("sp", (P(nc.sync.dma_start, out=xr[:], in_=resid_src(l, b, tt))), r=[state["rtok"][b][tt]], w=[t_xr], dma=True, key=xkey)
                        for db in range(4):
                            pt, ptk = psnext()
                            for c in range(KC):
                                A("pe", (P(nc.tensor.matmul, pt[:, :], lhsT=ynb[:, c, tj * 128:(tj + 1) * 128], rhs=wo[:, c, db * 512:(db + 1) * 512], start=(c == 0), stop=(c == KC - 1))), r=[t_ynb, t_wo], w=[ptk])
                            h2, t_h2 = h2R.next()
                            A("dve", (P(nc.vector.tensor_tensor, out=h2[:, :], in0=pt[:, :], in1=gbc[:, db * 512:(db + 1) * 512], op=ALU.mult)), r=[ptk, t_gbc], w=[t_h2])
                            A("pool", (P(nc.gpsimd.tensor_tensor, out=xr[:, db * 512:(db + 1) * 512], in0=xr[:, db * 512:(db + 1) * 512], in1=h2[:, :], op=ALU.add)), r=[t_h2, t_xr], w=[t_xr])
                        nt_ = tk("R"); state["rtok"][b][tt] = nt_
                        A("sp", (P(nc.sync.dma_start, out=R[b, tt * 128:(tt + 1) * 128, :], in_=xr[:])), r=[t_xr], w=[nt_], dma=True, key=xkey + "s")

                def d1_items():
                    for b in range(NB):
                        for (t0, n) in tgs:
                            if last and t0 >= S:
                                continue
                            r3 = b if t0 < S else 2
                            gk = "g%d" % r3
                            if gk not in gbcs:
                                gbcs[gk] = [psb("gbc" + gk, [128, D]), tk(), None]
                            if gbcs[gk][2] != r3:
                                gbcs[gk][2] = r3
                                A("sp", (P(nc.sync.dma_start, out=gbcs[gk][0][:], in_=MOD[l, r3, 2 * D:3 * D].partition_broadcast(128))), r=[t_MODs[l]], w=[gbcs[gk][1]], dma=True, key="gbc" + gk)
                            gbc, t_gbc = gbcs[gk][0], gbcs[gk][1]
                            yield d1_item(b, t0, n, gbc, t_gbc)
                pipeline(d1_items())
            with ExitStack() as ph:
                sch.barrier()
                def psb(name, shape, dt=F32):
                    return ph.enter_context(nc.sbuf_tensor(uname(name), shape, dt))
                wrt = psb("wrt", [128, KC, 36]); t_wrt = tk()
                A("sp", P(nc.sync.dma_start, out=wrt[:], in_=wr_d[l].rearrange("(c p) n -> p c n", p=128)), w=[t_wrt], dma=True, key="wrt")
                rbt = psb("rbt", [128, 36]); t_rbt = tk()
                A("sp", P(nc.sync.dma_start, out=rbt[:], in_=rb_d[l, 0].partition_broadcast(128)), w=[t_rbt], dma=True, key="rbt")
                xrR = Rot(psb, tk, "xr2", [128, D], F32, 3)
                h2R = Rot(psb, tk, "h2", [128, D], F32, 3)
                h2TR = Rot(psb, tk, "h2T", [128, KC, 128], F32, 3)
                h2bR = Rot(psb, tk, "h2b", [128, D], BF16, 2)
                junkR = Rot(psb, tk, "junkd", [128, D], BF16, 2)
                stR = Rot(psb, tk, "std", [128, 4], F32, 4)
                lgR = Rot(psb, tk, "lg", [128, 36], F32, 3)
                rtR = Rot(psb, tk, "rt", [128, 64], F32, 4)
                selR = Rot(psb, tk, "sel", [128, 8], F32, 2)
                top8R = Rot(psb, tk, "top8", [128, 8], F32, 2)
                ogR = Rot(psb, tk, "og", [128, 4], F32, 4)
                ohR = Rot(psb, tk, "oh", [128, 2, 8], F32, 4)
                mbR = Rot(psb, tk, "mb", [128, 32], BF16, 4)
                maccbR = Rot(psb, tk, "maccb", [128, 32], BF16, 3)
                cumR = Rot(psb, tk, "cum", [128, 32], F32, 3)

                def d2_item(b, tt):
                    gti = b * NTM + tt
                    r3 = b if tt < S // 128 else 2
                    xr, t_xr = xrR.next(); xkey = "xr2_%d" % (xrR.i % 3)
                    junk, t_junk = junkR.next(); st, t_st = stR.next()
                    A("sp", (P(nc.sync.dma_start, out=xr[:], in_=R[b, tt * 128:(tt + 1) * 128, :])), r=[state["rtok"][b][tt]], w=[t_xr], dma=True, key=xkey)
                    A("act", (P(nc.scalar.activation, out=junk[:], in_=xr[:], func=AF.Square, accum_out=st[:, 0:1])), r=[t_xr], w=[t_junk, t_st])
                    A("act", (P(nc.scalar.activation, out=st[:, 1:2], in_=st[:, 0:1], func=AF.Sqrt, scale=1.0 / D, bias=eps_t[:, 0:1])), r=[t_st, t_eps], w=[t_st])
                    A("dve", (P(nc.vector.reciprocal, out=st[:, 2:3], in_=st[:, 1:2])), r=[t_st], w=[t_st])
                    yield
                    h2, t_h2 = h2R.next()
                    A("act", (P(nc.scalar.activation, out=h2[:], in_=xr[:], func=AF.Identity, scale=st[:, 2:3])), r=[t_xr, t_st], w=[t_h2])
                    yield
                    pts = []
                    for c0 in range(0, KC, 4):
                        pt, ptk = psnext(); pts.append((pt, ptk))
                        for c in range(c0, c0 + 4):
                            A("pe", (P(nc.tensor.transpose, pt[:, (c - c0) * 128:(c - c0 + 1) * 128], h2[:, c * 128:(c + 1) * 128], ident[:])), r=[t_h2, t_ident], w=[ptk])
                    h2T, t_h2T = h2TR.next()
                    if not hasattr(h2TR, "tok2"): h2TR.tok2 = {}
                    t_h2Tb = h2TR.tok2.setdefault(id(t_h2T), tk("h2Thi"))
                    for c0 in range(0, KC, 4):
                        pt, ptk = pts[c0 // 4]
                        for c in range(c0, c0 + 4):
                            sc_ap = Avec[l][1][:, r3 * KC + c:r3 * KC + c + 1]
                            bi_ap = modT[l][:, (3 * 16 + c) * 3 + r3:(3 * 16 + c) * 3 + r3 + 1]
                            if c < 8:
                                A("dve", (P(nc.vector.tensor_scalar, out=h2T[:, c, :], in0=pt[:, (c - c0) * 128:(c - c0 + 1) * 128], scalar1=sc_ap, scalar2=bi_ap, op0=ALU.mult, op1=ALU.add)), r=[ptk, t_Av[l], t_modT[l]], w=[t_h2T])
                            else:
                                A("act", (P(nc.scalar.activation, out=h2T[:, c, :], in_=pt[:, (c - c0) * 128:(c - c0 + 1) * 128], func=AF.Identity, scale=sc_ap, bias=bi_ap)), r=[ptk, t_Av[l], t_modT[l]], w=[t_h2Tb])
                    yield
                    pl, plk = psnext()
                    for c in range(KC):
                        A("pe", (P(nc.tensor.matmul, pl[:, 0:36], lhsT=h2T[:, c, :], rhs=wrt[:, c, :], start=(c == 0), stop=(c == KC - 1))), r=[t_h2T if c < 8 else t_h2Tb, t_wrt], w=[plk])
                    pts = []
                    for c0 in range(0, KC, 4):
                        pt, ptk = psnext(); pts.append((pt, ptk))
                        for c in range(c0, c0 + 4):
                            A("pe", (P(nc.tensor.transpose, pt[:, (c - c0) * 128:(c - c0 + 1) * 128], h2T[:, c, :], ident[:])), r=[t_h2T if c < 8 else t_h2Tb, t_ident], w=[ptk])
                    lg, t_lg = lgR.next(); rt, t_rt = rtR.next(); sel, t_sel = selR.next(); top8, t_top8 = top8R.next()
                    og, t_og = ogR.next(); oh, t_oh = ohR.next(); mb, t_mb = mbR.next()
                    A("dve", (P(nc.vector.tensor_tensor, out=lg[:], in0=pl[:, 0:36], in1=rbt[:], op=ALU.add)), r=[plk, t_rbt], w=[t_lg])
                    h2b, t_h2b = h2bR.next(); hkey = "h2b%d" % (h2bR.i % 2)
                    for c0 in range(0, KC, 4):
                        pt, ptk = pts[c0 // 4]
                        A("act", (P(nc.scalar.activation, func=AF.Copy, out=h2b[:, c0 * 128:(c0 + 4) * 128], in_=pt[:, :])), r=[ptk], w=[t_h2b])
                    A("sp", (P(nc.sync.dma_start, out=H2[gti * 128:(gti + 1) * 128, :], in_=h2b[:])), r=[t_h2b], w=[t_H2], dma=True, key=hkey)
                    yield
                    A("dve", (P(nc.vector.tensor_reduce, out=rt[:, 0:1], in_=lg[:, 0:4], axis=AX.X, op=ALU.max)), r=[t_lg], w=[t_rt])
                    A("dve", (P(nc.vector.tensor_scalar, out=og[:], in0=lg[:, 0:4], scalar1=rt[:, 0:1], scalar2=None, op0=ALU.is_equal)), r=[t_lg, t_rt], w=[t_og])
                    A("dve", (P(nc.vector.tensor_scalar, out=rt[:, 1:2], in0=rt[:, 0:1], scalar1=-1.0, scalar2=None, op0=ALU.mult)), r=[t_rt], w=[t_rt])
                    A("act", (P(nc.scalar.activation, out=rt[:, 4:8], in_=lg[:, 0:4], func=AF.Exp, bias=rt[:, 1:2], accum_out=rt[:, 2:3])), r=[t_lg, t_rt], w=[t_rt])
                    A("dve", (P(nc.vector.tensor_scalar, out=sel[:], in0=lg[:, 4:12], scalar1=og[:, 0:1], scalar2=None, op0=ALU.mult)), r=[t_lg, t_og], w=[t_sel])
                    for g4 in range(1, 4):
                        A("dve", (P(nc.vector.scalar_tensor_tensor, out=sel[:], in0=lg[:, 4 + 8 * g4:12 + 8 * g4], scalar=og[:, g4:g4 + 1], in1=sel[:], op0=ALU.mult, op1=ALU.add)), r=[t_lg, t_og, t_sel], w=[t_sel])
                    A("dve", (P(nc.vector.max, out=top8[:], in_=sel[:])), r=[t_sel], w=[t_top8])
                    A("dve", (P(nc.vector.tensor_scalar, out=oh[:, 0, :], in0=sel[:], scalar1=top8[:, 0:1], scalar2=None, op0=ALU.is_equal)), r=[t_sel, t_top8], w=[t_oh])
                    A("dve", (P(nc.vector.tensor_scalar, out=oh[:, 1, :], in0=sel[:], scalar1=top8[:, 1:2], scalar2=None, op0=ALU.is_equal)), r=[t_sel, t_top8], w=[t_oh])
                    A("dve", (P(nc.vector.tensor_tensor, out=rt[:, 8:9], in0=top8[:, 1:2], in1=top8[:, 0:1], op=ALU.subtract)), r=[t_top8, t_rt], w=[t_rt])
                    A("act", (P(nc.scalar.activation, out=rt[:, 9:10], in_=rt[:, 8:9], func=AF.Exp)), r=[t_rt], w=[t_rt])
                    for g4 in range(4):
                        A("dve", (P(nc.vector.tensor_scalar, out=M1[:, gti, g4 * 8:(g4 + 1) * 8], in0=oh[:, 0, :], scalar1=og[:, g4:g4 + 1], scalar2=None, op0=ALU.mult)), r=[t_oh, t_og], w=[t_M])
                        A("dve", (P(nc.vector.tensor_scalar, out=M2[:, gti, g4 * 8:(g4 + 1) * 8], in0=oh[:, 1, :], scalar1=og[:, g4:g4 + 1], scalar2=None, op0=ALU.mult)), r=[t_oh, t_og], w=[t_M])
                    A("dve", (P(nc.vector.tensor_tensor, out=mb[:], in0=M1[:, gti, :], in1=M2[:, gti, :], op=ALU.add)), r=[t_M], w=[t_mb])
                    yield
                    A("dve", (P(nc.vector.reciprocal, out=rt[:, 3:4], in_=rt[:, 2:3])), r=[t_rt], w=[t_rt])
                    A("dve", (P(nc.vector.tensor_scalar, out=rt[:, 10:11], in0=rt[:, 9:10], scalar1=1.0, scalar2=None, op0=ALU.add)), r=[t_rt], w=[t_rt])
                    A("dve", (P(nc.vector.reciprocal, out=rt[:, 11:12], in_=rt[:, 10:11])), r=[t_rt], w=[t_rt])
                    A("dve", (P(nc.vector.tensor_tensor, out=RW[:, gti, 0:1], in0=rt[:, 11:12], in1=rt[:, 3:4], op=ALU.mult)), r=[t_rt], w=[t_RW])
                    A("dve", (P(nc.vector.tensor_tensor, out=RW[:, gti, 1:2], in0=rt[:, 3:4], in1=RW[:, gti, 0:1], op=ALU.subtract)), r=[t_rt, t_RW], w=[t_RW])
                    maccb, t_maccb = maccbR.next(); cum, t_cum = cumR.next()
                    pc_, pck_ = psnext()
                    A("dve", (P(nc.vector.tensor_copy, out=maccb[:], in_=macc[:])), r=[t_macc], w=[t_maccb])
                    A("dve", (P(nc.vector.tensor_tensor, out=macc[:], in0=macc[:], in1=mb[:], op=ALU.add)), r=[t_macc, t_mb], w=[t_macc])
                    A("pe", (P(nc.tensor.matmul, pc_[:, 0:32], lhsT=ustr[:], rhs=mb[:], start=True, stop=False)), r=[t_ustr, t_mb], w=[pck_])
                    A("pe", (P(nc.tensor.matmul, pc_[:, 0:32], lhsT=onesb[:], rhs=maccb[:], start=False, stop=True)), r=[t_onesb, t_maccb], w=[pck_])
                    yield
                    A("dve", (P(nc.vector.tensor_copy, out=cum[:], in_=pc_[:, 0:32])), r=[pck_], w=[t_cum])
                    A("dve", (P(nc.vector.tensor_tensor, out=rt[:, 16:48], in0=cum[:], in1=M1[:, gti, :], op=ALU.mult)), r=[t_cum, t_M, t_rt], w=[t_rt])
                    A("dve", (P(nc.vector.tensor_reduce, out=RK[:, gti, 0:1], in_=rt[:, 16:48], axis=AX.X, op=ALU.add)), r=[t_rt], w=[t_RK])
                    A("dve", (P(nc.vector.tensor_tensor, out=rt[:, 16:48], in0=cum[:], in1=M2[:, gti, :], op=ALU.mult)), r=[t_cum, t_M, t_rt], w=[t_rt])
                    A("dve", (P(nc.vector.tensor_reduce, out=RK[:, gti, 1:2], in_=rt[:, 16:48], axis=AX.X, op=ALU.add)), r=[t_rt], w=[t_RK])
                pipeline(d2_item(b, tt) for b in range(NB) for tt in range(NTM))
            if stop_after == "D" and l == 0:
                break
            NGT = NB * NTM
            cnt = psbm("cnt", [128, 32]); t_cnt = tk()
            pad = psbm("pad", [128, 32]); pstart = psbm("pstart", [128, 33]); t_pad = tk()
            padi = psbm("padi", [128, 32], I32); t_padi = tk()
            DSTf = psbm("DSTf", [128, NGT, 2]); DST = psbm("DST", [128, NGT, 2], U32); t_DST = tk()
            ebf = psbm("ebf", [128, NBLK]); t_ebf = tk()
            widx = psbm("widx", [128, NBLK, 3], U32); widxf = psbm("widxf", [128, NBLK, 3]); t_widx = tk()
            scr = psbm("scr", [128, 32]); t_scr = tk()
            pc_, pck_ = psnext()
            A("pe", (P(nc.tensor.matmul, pc_[:, 0:32], lhsT=onesf[:], rhs=macc[:], start=True, stop=True)), r=[t_onesf, t_macc], w=[pck_])
            A("dve", (P(nc.vector.tensor_scalar, out=cnt[:], in0=pc_[:, 0:32], scalar1=float(BLK - 1), scalar2=1.0 / BLK, op0=ALU.add, op1=ALU.mult)), r=[pck_], w=[t_cnt])
            A("dve", (P(nc.vector.tensor_copy, out=padi[:], in_=cnt[:])), r=[t_cnt], w=[t_padi])
            A("dve", (P(nc.vector.tensor_copy, out=pad[:], in_=padi[:])), r=[t_padi], w=[t_pad])
            A("dve", (P(nc.vector.tensor_tensor, out=scr[:], in0=pad[:], in1=cnt[:], op=ALU.is_gt)), r=[t_pad, t_cnt], w=[t_scr])
            A("dve", (P(nc.vector.tensor_tensor, out=pad[:], in0=pad[:], in1=scr[:], op=ALU.subtract)), r=[t_pad, t_scr], w=[t_pad])
            A("dve", (P(nc.vector.tensor_scalar, out=pad[:], in0=pad[:], scalar1=float(BLK), scalar2=None, op0=ALU.mult)), r=[t_pad], w=[t_pad])
            A("dve", (P(nc.vector.memset, pstart[:, 0:1], 0.0)), w=[t_pad])
            for e in range(32):
                A("dve", (P(nc.vector.tensor_tensor, out=pstart[:, e + 1:e + 2], in0=pstart[:, e:e + 1], in1=pad[:, e:e + 1], op=ALU.add)), r=[t_pad], w=[t_pad])
            for gti in range(NGT):
                for k, Mk in enumerate((M1, M2)):
                    A("dve", (P(nc.vector.tensor_tensor, out=scr[:], in0=pstart[:, 0:32], in1=Mk[:, gti, :], op=ALU.mult)), r=[t_pad, t_M], w=[t_scr])
                    A("dve", (P(nc.vector.tensor_reduce, out=DSTf[:, gti, k:k + 1], in_=scr[:], axis=AX.X, op=ALU.add)), r=[t_scr], w=[t_DST])
            A("dve", (P(nc.vector.tensor_tensor, out=DSTf[:], in0=DSTf[:], in1=RK[:], op=ALU.add)), r=[t_DST, t_RK], w=[t_DST])
            A("dve", (P(nc.vector.tensor_copy, out=DST[:], in_=DSTf[:])), r=[t_DST], w=[t_DST])
            for bk in range(NBLK):
                A("dve", (P(nc.vector.tensor_scalar, out=scr[:], in0=pstart[:, 1:33], scalar1=float(bk * BLK), scalar2=None, op0=ALU.is_le)), r=[t_pad], w=[t_scr])
                A("dve", (P(nc.vector.tensor_reduce, out=ebf[:, bk:bk + 1], in_=scr[:], axis=AX.X, op=ALU.add)), r=[t_scr], w=[t_ebf])
            A("dve", (P(nc.vector.tensor_scalar, out=ebf[:], in0=ebf[:], scalar1=31.0, scalar2=None, op0=ALU.min)), r=[t_ebf], w=[t_ebf])
            A("dve", (P(nc.vector.tensor_scalar, out=widxf[:, :, 0], in0=ebf[:], scalar1=256.0, scalar2=pidx[:, 0:1], op0=ALU.mult, op1=ALU.add)), r=[t_ebf, t_pidx], w=[t_widx])
            A("dve", (P(nc.vector.tensor_scalar, out=widxf[:, :, 1], in0=widxf[:, :, 0], scalar1=128.0, scalar2=None, op0=ALU.add)), r=[t_widx], w=[t_widx])
            A("dve", (P(nc.vector.tensor_scalar, out=widxf[:, :, 2], in0=ebf[:], scalar1=128.0, scalar2=pidx[:, 0:1], op0=ALU.mult, op1=ALU.add)), r=[t_ebf, t_pidx], w=[t_widx])
            A("dve", (P(nc.vector.tensor_scalar, out=widxf[:, :, 0:2], in0=widxf[:, :, 0:2], scalar1=float(l * NE * 256), scalar2=None, op0=ALU.add)), r=[t_widx], w=[t_widx])
            A("dve", (P(nc.vector.tensor_scalar, out=widxf[:, :, 2], in0=widxf[:, :, 2], scalar1=float(l * NE * 128), scalar2=None, op0=ALU.add)), r=[t_widx], w=[t_widx])
            A("dve", (P(nc.vector.tensor_copy, out=widx[:], in_=widxf[:])), r=[t_widx], w=[t_widx])
            t_XSs = []
            with ExitStack() as ph:
                sch.barrier()
                def psb(name, shape, dt=F32):
                    return ph.enter_context(nc.sbuf_tensor(uname(name), shape, dt))
                hbR = Rot(psb, tk, "hb", [128, D], BF16, 2)
                def e_item(gti):
                    hb, t_hb = hbR.next(); hk = "hb%d" % (hbR.i % 2)
                    A("sp", (P(nc.sync.dma_start, out=hb[:], in_=H2[gti * 128:(gti + 1) * 128, :])), r=[t_H2], w=[t_hb], dma=True, key=hk)
                    yield
                    for k in range(2):
                        A("pool", (P(nc.gpsimd.indirect_dma_start, out=XS, out_offset=bass.IndirectOffsetOnAxis(ap=DST[:, gti, k:k + 1], axis=0), in_=hb[:], in_offset=None)), r=[t_hb, t_DST], w=[t_XSs.append(tk("XS")) or t_XSs[-1]], dma=True, key=hk + "s%d" % k)
                pipeline(e_item(gti) for gti in range(NGT))
            t_YSs = []
            with ExitStack() as ph:
                sch.barrier()
                def psb(name, shape, dt=F32):
                    return ph.enter_context(nc.sbuf_tensor(uname(name), shape, dt))
                xbR = Rot(psb, tk, "xb", [128, 2, D], BF16, 2)
                xTR = Rot(psb, tk, "xTe", [128, KC, BLK], BF16, 2)
                wgR = Rot(psb, tk, "wgt", [128, KC * 256], BF16, 4)
                wuR = Rot(psb, tk, "wut", [128, KC * 256], BF16, 4)
                wdR = Rot(psb, tk, "wdt", [128, 4 * D], BF16, 2)
                sgR = Rot(psb, tk, "sg", [128, BLK], F32, 3)
                aTR = Rot(psb, tk, "aT", [128, 4, BLK], BF16, 2)
                ybR = Rot(psb, tk, "yb", [128, D], F32, 2)
                wgf = wg_d.rearrange("l r c -> (l r) c"); wuf = wu_d.rearrange("l r c -> (l r) c"); wdf = wd_d.rearrange("l r c -> (l r) c")

                def f_item(bk):
                    xb, t_xb = xbR.next(); xkey = "xb%d" % (xbR.i % 2)
                    A("sp", (P(nc.sync.dma_start, out=xb[:], in_=XS[bk * BLK:(bk + 1) * BLK, :].rearrange("(j p) d -> p j d", p=128))), r=t_XSs, w=[t_xb], dma=True, key=xkey)
                    yield
                    wd_, t_wd_ = wdR.next(); dkey = "wdt%d" % (wdR.i % 2)
                    A("pool", (P(nc.gpsimd.indirect_dma_start, out=wd_[:], out_offset=None, in_=wdf, in_offset=bass.IndirectOffsetOnAxis(ap=widx[:, bk, 2:3], axis=0))), r=[t_widx], w=[t_wd_], dma=True, key=dkey)
                    halves = []
                    for fh in range(2):
                        wg_, t_wg_ = wgR.next(); gkey = "wgt%d" % (wgR.i % 4)
                        wu_, t_wu_ = wuR.next(); ukey = "wut%d" % (wuR.i % 4)
                        A("pool", (P(nc.gpsimd.indirect_dma_start, out=wg_[:], out_offset=None, in_=wgf, in_offset=bass.IndirectOffsetOnAxis(ap=widx[:, bk, fh:fh + 1], axis=0))), r=[t_widx], w=[t_wg_], dma=True, key=gkey)
                        A("pool", (P(nc.gpsimd.indirect_dma_start, out=wu_[:], out_offset=None, in_=wuf, in_offset=bass.IndirectOffsetOnAxis(ap=widx[:, bk, fh:fh + 1], axis=0))), r=[t_widx], w=[t_wu_], dma=True, key=ukey)
                        halves.append((wg_, t_wg_, wu_, t_wu_))
                    xT, t_xT = xTR.next()
                    for j in range(2):
                        for c0 in range(0, KC, 8):
                            pt, ptk = psnext()
                            ptb = pt[:].bitcast(BF16)
                            for c in range(c0, c0 + 8):
                                A("pe", (P(nc.tensor.transpose, ptb[:, (c - c0) * 128:(c - c0 + 1) * 128], xb[:, j, c * 128:(c + 1) * 128], identb[:])), r=[t_xb, t_identb], w=[ptk])
                            if (j + c0 // 8) % 2 == 0:
                                A("act", (P(nc.scalar.activation, func=AF.Copy, out=xT[:, c0:c0 + 8, j * 128:(j + 1) * 128], in_=ptb[:, 0:1024].rearrange("p (c t) -> p c t", t=128))), r=[ptk], w=[t_xT])
                            else:
                                A("dve", (P(nc.vector.tensor_copy, out=xT[:, c0:c0 + 8, j * 128:(j + 1) * 128], in_=ptb[:, 0:1024].rearrange("p (c t) -> p c t", t=128))), r=[ptk], w=[t_xT])
                    yield
                    aT, t_aT = aTR.next()
                    for fh in range(2):
                        wg_, t_wg_, wu_, t_wu_ = halves[fh]
                        for fl in range(2):
                            fc = fh * 2 + fl
                            pg, pgk = psnext(); pu, puk = psnext()
                            for c in range(KC):
                                A("pe", (P(nc.tensor.matmul, pg[:, 0:BLK], lhsT=wg_[:, c * 256 + fl * 128: c * 256 + (fl + 1) * 128], rhs=xT[:, c, :], start=(c == 0), stop=(c == KC - 1))), r=[t_wg_, t_xT], w=[pgk])
                            for c in range(KC):
                                A("pe", (P(nc.tensor.matmul, pu[:, 0:BLK], lhsT=wu_[:, c * 256 + fl * 128: c * 256 + (fl + 1) * 128], rhs=xT[:, c, :], start=(c == 0), stop=(c == KC - 1))), r=[t_wu_, t_xT], w=[puk])
                            sg, t_sg = sgR.next()
                            A("act", (P(nc.scalar.activation, out=sg[:], in_=pg[:, 0:BLK], func=AF.Silu)), r=[pgk], w=[t_sg])
                            A("dve", (P(nc.vector.tensor_tensor, out=aT[:, fc, :], in0=sg[:], in1=pu[:, 0:BLK], op=ALU.mult)), r=[t_sg, puk], w=[t_aT])
                    for j in range(2):
                        yb, t_yb = ybR.next(); ykey = "yb%d" % (ybR.i % 2)
                        for db in range(4):
                            pt, ptk = psnext()
                            for fc in range(4):
                                A("pe", (P(nc.tensor.matmul, pt[:, :], lhsT=aT[:, fc, j * 128:(j + 1) * 128], rhs=wd_[:, fc * D + db * 512: fc * D + (db + 1) * 512], start=(fc == 0), stop=(fc == 3))), r=[t_aT, t_wd_], w=[ptk])
                            if db % 2 == 0:
                                A("act", (P(nc.scalar.activation, func=AF.Copy, out=yb[:, db * 512:(db + 1) * 512], in_=pt[:, :])), r=[ptk], w=[t_yb])
                            else:
                                A("dve", (P(nc.vector.tensor_copy, out=yb[:, db * 512:(db + 1) * 512], in_=pt[:, :])), r=[ptk], w=[t_yb])
                        A("sp", (P(nc.sync.dma_start, out=YS[bk * BLK + j * 128: bk * BLK + (j + 1) * 128, :], in_=yb[:])), r=[t_yb], w=[t_YSs.append(tk("YS")) or t_YSs[-1]], dma=True, key=ykey)
                pipeline(f_item(bk) for bk in range(NBLK))
            with ExitStack() as ph:
                sch.barrier()
                def psb(name, shape, dt=F32):
                    return ph.enter_context(nc.sbuf_tensor(uname(name), shape, dt))
                y0R = Rot(psb, tk, "y0", [128, D], F32, 3)
                y1R = Rot(psb, tk, "y1", [128, D], F32, 3)
                xgR = Rot(psb, tk, "xg", [128, D], F32, 4)
                junkR = Rot(psb, tk, "junkg", [128, D], BF16, 2)
                stR = Rot(psb, tk, "stg", [128, 4], F32, 3)
                g2s = {}
                if last:
                    fgb = psb("fgb", [128, D]); t_fgb = tk()
                    A("sp", P(nc.sync.dma_start, out=fgb[:], in_=fg_d[0].partition_broadcast(128)), w=[t_fgb], dma=True, key="fgb")

                def g_item(b, tt, g2, t_g2):
                    gti = b * NTM + tt
                    y0, t_y0 = y0R.next(); k0_ = "y0_%d" % (y0R.i % 3)
                    y1, t_y1 = y1R.next(); k1_ = "y1_%d" % (y1R.i % 3)
                    xr, t_xr = xgR.next(); kx_ = "xg%d" % (xgR.i % 4)
                    A("pool", (P(nc.gpsimd.indirect_dma_start, out=y0[:], out_offset=None, in_=YS, in_offset=bass.IndirectOffsetOnAxis(ap=DST[:, gti, 0:1], axis=0))), r=t_YSs + [t_DST], w=[t_y0], dma=True, key=k0_)
                    A("pool", (P(nc.gpsimd.indirect_dma_start, out=y1[:], out_offset=None, in_=YS, in_offset=bass.IndirectOffsetOnAxis(ap=DST[:, gti, 1:2], axis=0))), r=t_YSs + [t_DST], w=[t_y1], dma=True, key=k1_)
                    A("sp", (P(nc.sync.dma_start, out=xr[:], in_=R[b, tt * 128:(tt + 1) * 128, :])), r=[state["rtok"][b][tt]], w=[t_xr], dma=True, key=kx_)
                    yield
                    yield
                    A("act", (P(nc.scalar.activation, out=y0[:], in_=y0[:], func=AF.Identity, scale=RW[:, gti, 0:1])), r=[t_y0, t_RW], w=[t_y0])
                    A("dve", (P(nc.vector.scalar_tensor_tensor, out=y0[:], in0=y1[:], scalar=RW[:, gti, 1:2], in1=y0[:], op0=ALU.mult, op1=ALU.add)), r=[t_y0, t_y1, t_RW], w=[t_y0])
                    A("pool", (P(nc.gpsimd.tensor_tensor, out=y0[:], in0=y0[:], in1=g2[:], op=ALU.mult)), r=[t_y0, t_g2], w=[t_y0])
                    A("dve", (P(nc.vector.tensor_tensor, out=xr[:], in0=xr[:], in1=y0[:], op=ALU.add)), r=[t_y0, t_xr], w=[t_xr])
                    if last:
                        junk, t_junk = junkR.next(); st, t_st = stR.next()
                        A("act", (P(nc.scalar.activation, out=junk[:], in_=xr[:], func=AF.Square, accum_out=st[:, 0:1])), r=[t_xr], w=[t_junk, t_st])
                        A("act", (P(nc.scalar.activation, out=st[:, 1:2], in_=st[:, 0:1], func=AF.Sqrt, scale=1.0 / D, bias=eps_t[:, 0:1])), r=[t_st, t_eps], w=[t_st])
                        A("dve", (P(nc.vector.reciprocal, out=st[:, 2:3], in_=st[:, 1:2])), r=[t_st], w=[t_st])
                        A("dve", (P(nc.vector.scalar_tensor_tensor, out=xr[:], in0=xr[:], scalar=st[:, 2:3], in1=fgb[:], op0=ALU.mult, op1=ALU.mult)), r=[t_xr, t_st, t_fgb], w=[t_xr])
                    yield
                    if not last:
                        nt_ = tk("R"); state["rtok"][b][tt] = nt_
                        A("sp", (P(nc.sync.dma_start, out=R[b, tt * 128:(tt + 1) * 128, :], in_=xr[:])), r=[t_xr], w=[nt_], dma=True, key=kx_ + "s")
                    else:
                        nt_ = tk("O")
                        A("sp", (P(nc.sync.dma_start, out=out_d[b, tt * 128:(tt + 1) * 128, :], in_=xr[:])), r=[t_xr], w=[nt_], dma=True, key=kx_ + "s")

                def g_items():
                    for b in range(NB):
                        for tt in range(NTM):
                            r3 = b if tt < S // 128 else 2
                            if r3 not in g2s:
                                g2 = psb("g2_%d" % r3, [128, D]); t_g2 = tk()
                                A("sp", (P(nc.sync.dma_start, out=g2[:], in_=MOD[l, r3, 5 * D:6 * D].partition_broadcast(128))), r=[t_MODs[l]], w=[t_g2], dma=True, key="g2_%d" % r3)
                                g2s[r3] = (g2, t_g2)
                            yield g_item(b, tt, *g2s[r3])
                pipeline(g_items())
        if stop_after is not None and l == 0:
            break

    if dbg:
        with ExitStack() as ph:
            sch.barrier()
            dt_ = ph.enter_context(nc.sbuf_tensor(uname("dbgt"), [128, D], F32)); t_dt = tk()
            for b in range(NB):
                for tt in range(NTT):
                    A("sp", (P(nc.sync.dma_start, out=dt_[:], in_=R[b, tt * 128:(tt + 1) * 128, :])), r=[state["rtok"][b][tt]], w=[t_dt], dma=True, key="dbgl")
                    A("sp", (P(nc.sync.dma_start, out=dbg_d[b, tt * 128:(tt + 1) * 128, :], in_=dt_[:])), r=[t_dt], w=[tk()], dma=True, key="dbgs")
    nsem = sch.emit()
    es.close()
    return nc


def _fm(v):
    return np.ascontiguousarray(v.reshape(-1, 128).T)


def prep_shared(inp, S):
    sh = {}
    sh["w_ada"] = np.ascontiguousarray(inp["w_ada"]); sh["b_ada"] = np.ascontiguousarray(inp["b_ada"][:, None, :])
    pvs = []
    for l in range(2):
        pv = np.zeros((128, 256), np.float32)
        pv[:, 0:16] = _fm(inp["norm1_g"][l]); pv[:, 16:32] = _fm(inp["norm2_g"][l]); pv[:, 32:48] = _fm(inp["grp_norm_g"][l])
        pv[:, 48] = inp["q_norm_g"][l]; pv[:, 49] = inp["k_norm_g"][l]
        for k in range(3):
            pv[:, 50 + k * 4: 50 + k * 4 + 4] = _fm(inp["sconv_w"][l, k])
        for k in range(31):
            pv[:, 62 + k * 4: 62 + k * 4 + 4] = _fm(inp["conf_dw_w"][l, k])
        pv[:, 186:190] = _fm(inp["conf_dw_b"][l]); pv[:, 190:194] = _fm(inp["conf_ln_g"][l]); pv[:, 194:198] = _fm(inp["conf_ln_b"][l])
        pvs.append(pv)
    sh["pv"] = np.stack(pvs)
    sh["fg"] = np.ascontiguousarray(inp["final_g"][None, :])
    cols = list(range(0, 1024)) + list(range(1024, 1536))
    for j in range(4):
        cols += list(range(1536 + j * 128, 1536 + (j + 1) * 128))
        cols += list(range(2048 + j * 128, 2048 + (j + 1) * 128))
        cols += list(range(2560 + j * 128, 2560 + (j + 1) * 128))
    for j in range(4):
        cols += list(range(3072 + j * 128, 3072 + (j + 1) * 128))
        cols += list(range(3584 + j * 128, 3584 + (j + 1) * 128))
    sh["w_in"] = np.ascontiguousarray(inp["w_in"][:, :, cols])
    sh["w_out"] = np.ascontiguousarray(inp["w_out"])
    sh["wr"] = np.ascontiguousarray(np.concatenate([inp["router_g_w"], inp["router_e_w"]], axis=-1))
    sh["rb"] = np.ascontiguousarray(np.concatenate([inp["router_g_b"], inp["router_e_b"]], axis=-1)[:, None, :])
    for nm, key in (("wg", "exp_w_gate"), ("wu", "exp_w_up")):
        w = inp[key].reshape(2, NE, KC, 128, 2, 256)
        sh[nm] = np.ascontiguousarray(w.transpose(0, 1, 4, 3, 2, 5)).reshape(2, NE * 2 * 128, KC * 256)
    w = inp["exp_w_down"].reshape(2, NE, 4, 128, D)
    sh["wd"] = np.ascontiguousarray(w.transpose(0, 1, 3, 2, 4)).reshape(2, NE * 128, 4 * D)
    rows = S // GRID_W
    row = np.repeat(np.arange(rows), GRID_W).astype(np.float32); col = np.tile(np.arange(GRID_W), rows).astype(np.float32)
    inv = (10000.0 ** (-np.arange(0, 64, 2, dtype=np.float32) / 64)).astype(np.float32)
    ar = row[:, None] * inv; ac = col[:, None] * inv
    ang = np.concatenate([ar, ar, ac, ac], axis=-1)
    sgn = np.concatenate([-np.ones(32), np.ones(32), -np.ones(32), np.ones(32)]).astype(np.float32)
    pm = np.zeros((128, 128), np.float32)
    for m in range(128):
        pm[m + 32 if (m % 64) < 32 else m - 32, m] = 1.0
    sh["perm"] = pm
    sh["cs"] = np.ascontiguousarray(np.stack([np.cos(ang).T, (np.sin(ang) * sgn[None, :]).T]).astype(np.float32))
    return sh


def make_in_maps(inp, S, L, NB, ncores):
    inp = {k: np.asarray(v) for k, v in inp.items()}
    sh = prep_shared(inp, S)
    maps = []
    for ci in range(ncores):
        m = dict(sh)
        m["x"] = np.ascontiguousarray(inp["x"][ci * NB:(ci + 1) * NB])
        m["ctx"] = np.ascontiguousarray(inp["ctx"][ci * NB:(ci + 1) * NB])
        rows3 = np.stack([inp["c"][ci * NB + r] if r < NB else inp["c_ctx"] for r in range(3)]) if NB == 2 else None
        if NB == 1:
            rows3 = np.stack([inp["c"][ci], inp["c"][ci], inp["c_ctx"]])
        cT = rows3.reshape(3, KC, 128).transpose(2, 1, 0).reshape(128, KC * 3)
        m["cT"] = np.ascontiguousarray(cT)
        maps.append(m)
    return maps


_CACHE = {}


def kernel(**inputs):
    B, S, _ = inputs["x"].shape
    L = inputs["ctx"].shape[1]
    ncores = 8; NB = B // ncores
    key = (S, L, NB)
    if key not in _CACHE:
        _CACHE[key] = build(S, L, NB)
    nc = _CACHE[key]
    maps = make_in_maps(inputs, S, L, NB, ncores)
    res = run_bass_kernel_spmd(nc, maps, core_ids=list(range(ncores)))
    return np.concatenate([r["out"] for r in res.results], axis=0).astype(np.float32)
```
